# Optimizing a Trainium2 kernel written in Bass

```python
import math
import jax, jax.numpy as jnp
from jax import lax
import numpy as np

D_MODEL = 1024
BATCH = 16
SEQ = 2048
DEPTH = 2

RET_HEADS = 4
RET_DK = 128
RET_DV = 256
RET_CHUNK = 128
ROPE_BASE = 10000.0
GN_EPS = 1e-5
DIFF_HEADS = 8
DIFF_DH = 64
DIFF_QBLOCK = 128
DIFF_EPS = 1e-5
DIL_PATTERNS = ((128, 1), (512, 4), (2048, 16))
N_DIL = len(DIL_PATTERNS)
DIL_HEADS = 4
DIL_DH = 128
DIL_BLOCK = 128
REL_BUCKETS = 32
REL_MAX_DIST = 128
N_BIAS_HEADS = DIFF_HEADS + N_DIL * DIL_HEADS
N_BRANCHES = 3
FFN_HIDDEN = -(-8 * D_MODEL // (3 * 256)) * 256
RMS_EPS = 1e-6
NEG_INF = -1e30
IN_SPLITS = ((RET_HEADS * RET_DK,) * 2 + (RET_HEADS * RET_DV,) * 2 + (DIFF_HEADS * 2 * DIFF_DH,) * 3
             + (DIL_HEADS * DIL_DH,) * (3 * N_DIL) + (N_BRANCHES * D_MODEL,))
IN_WIDTH = sum(IN_SPLITS)

kernel_name = "hybrid_retention_diffattn_dilated_block"


def rmsnorm(x, g, eps=RMS_EPS):
    xf = x.astype(jnp.float32)
    y = xf * lax.rsqrt(jnp.mean(xf * xf, axis=-1, keepdims=True) + eps)
    return (y * g.astype(jnp.float32)).astype(x.dtype)


def rel_bucket(dist):
    n = jnp.maximum(dist, 0)
    exact = REL_BUCKETS // 2
    log_ratio = jnp.log(jnp.maximum(n, exact).astype(jnp.float32) / exact) / math.log(REL_MAX_DIST / exact)
    large = jnp.minimum(exact + (log_ratio * (REL_BUCKETS - exact)).astype(jnp.int32), REL_BUCKETS - 1)
    return jnp.where(n < exact, n, large)


def rotary(x, pos):
    half = x.shape[-1] // 2
    inv = ROPE_BASE ** (-jnp.arange(half, dtype=jnp.float32) / half)
    ang = pos[:, None] * inv[None, :]
    cos, sin = jnp.cos(ang)[:, None, :], jnp.sin(ang)[:, None, :]
    x1, x2 = x[..., :half], x[..., half:]
    return jnp.concatenate([x1 * cos - x2 * sin, x1 * sin + x2 * cos], axis=-1)


def retention(q, k, v, g, gn_gain):
    b, S = q.shape[0], q.shape[1]
    H, dk, dv, c = RET_HEADS, RET_DK, RET_DV, RET_CHUNK
    n = S // c
    pos = jnp.arange(S, dtype=jnp.float32)
    q = rotary(q, pos).reshape(b, n, c, H, dk)
    k = (rotary(k, pos) * (dk ** -0.5)).reshape(b, n, c, H, dk)
    vv = v.reshape(b, n, c, H, dv)
    log_gamma = jnp.log1p(-jnp.exp2(-5.0 - jnp.arange(H, dtype=jnp.float32)))
    i = jnp.arange(c, dtype=jnp.float32)
    rel = i[:, None] - i[None, :]
    decay = jnp.where(rel >= 0, jnp.exp(log_gamma[:, None, None] * jnp.maximum(rel, 0.0)), 0.0)
    scores = jnp.einsum('bnihd,bnjhd->bnhij', q, k) * decay
    y_intra = jnp.einsum('bnhij,bnjhe->bnihe', scores, vv)
    k_to_end = jnp.exp((c - 1.0 - i)[:, None] * log_gamma[None, :])
    kv = jnp.einsum('bnjhd,bnjhe->nbhde', k * k_to_end[:, :, None], vv).astype(jnp.float32)
    chunk_decay = jnp.exp(c * log_gamma)[:, None, None]

    def step(state, kv_n):
        return state * chunk_decay + kv_n, state

    _, state_prev = lax.scan(step, jnp.zeros(kv.shape[1:], jnp.float32), kv)
    q_from_start = jnp.exp((i + 1.0)[:, None] * log_gamma[None, :])
    y_cross = jnp.einsum('bnihd,nbhde->bnihe', q * q_from_start[:, :, None], state_prev)
    y = (y_intra + y_cross).reshape(b, S, H, dv).astype(jnp.float32)
    mu = jnp.mean(y, axis=-1, keepdims=True)
    var = jnp.mean(jnp.square(y - mu), axis=-1, keepdims=True)
    y = ((y - mu) * lax.rsqrt(var + GN_EPS)).reshape(b, S, H * dv) * gn_gain.astype(jnp.float32)
    return (jax.nn.silu(g.astype(jnp.float32)) * y).astype(v.dtype)


def diff_attention(q, k, v, lam_params, lam_init, subln_gain, bias_table):
    b, S, H, _, dh = q.shape
    QB = DIFF_QBLOCK
    nb = S // QB
    lp = lam_params.astype(jnp.float32)
    lam = jnp.exp(jnp.sum(lp[0] * lp[1])) - jnp.exp(jnp.sum(lp[2] * lp[3])) + lam_init
    q_blocks = jnp.moveaxis(q.reshape(b, nb, QB, H, 2, dh), 1, 0)
    k_pos = jnp.arange(S)
    vf = v.astype(jnp.float32)

    def one_block(args):
        qblk, blk = args
        dist = (blk * QB + jnp.arange(QB))[:, None] - k_pos[None, :]
        bias = jnp.moveaxis(bias_table[rel_bucket(dist)].astype(jnp.float32), -1, 0)
        bias = jnp.where(dist >= 0, bias, NEG_INF)
        s = jnp.einsum('bqhmd,bkhmd->bhmqk', qblk, k).astype(jnp.float32) * (dh ** -0.5) + bias[:, None]
        p = jax.nn.softmax(s, axis=-1)
        a = p[:, :, 0] - lam * p[:, :, 1]
        return jnp.einsum('bhqk,bkhe->bqhe', a, vf)

    o = lax.map(one_block, (q_blocks, jnp.arange(nb)))
    o = jnp.moveaxis(o, 0, 1).reshape(b, S, H, 2 * dh)
    o = rmsnorm(o, subln_gain, DIFF_EPS) * (1.0 - lam_init)
    return o.reshape(b, S, H * 2 * dh).astype(v.dtype)


def dilated_group(q, k, v, window, dilation, bias_table):
    b, S, H, dh = q.shape
    L = DIL_BLOCK
    n_sub = S // dilation
    nb = -(-n_sub // L)
    n_pad = nb * L

    def to_sub(t):
        t = jnp.swapaxes(t.reshape(b, n_sub, dilation, H, dh), 1, 2)
        return jnp.pad(t, ((0, 0), (0, 0), (0, n_pad - n_sub), (0, 0), (0, 0)))

    def kv_band(t):
        prev = jnp.pad(t, ((0, 0), (0, 0), (L, 0), (0, 0), (0, 0)))[:, :, :n_pad]
        return jnp.concatenate([prev.reshape(b, dilation, nb, L, H, dh),
                                t.reshape(b, dilation, nb, L, H, dh)], axis=3)

    qb = to_sub(q).reshape(b, dilation, nb, L, H, dh)
    kb = kv_band(to_sub(k))
    vb = kv_band(to_sub(v)).astype(jnp.float32)
    qi = jnp.arange(L)
    kj = jnp.arange(2 * L)
    m = qi[:, None] + L - kj[None, :]
    key_sub = (jnp.arange(nb) * L)[:, None] - L + kj[None, :]
    mask = ((m >= 0) & (m <= window // dilation))[None] & (key_sub >= 0)[:, None, :]
    bias = jnp.transpose(bias_table[rel_bucket(m * dilation)], (2, 0, 1)).astype(jnp.float32)
    s = jnp.einsum('bdnqhe,bdnkhe->bdnhqk', qb, kb).astype(jnp.float32) * (dh ** -0.5) + bias
    s = jnp.where(mask[:, None], s, NEG_INF)
    mx = jnp.max(s, axis=-1, keepdims=True)
    p = jnp.exp(s - mx)
    den = jnp.sum(p, axis=-1)
    num = jnp.einsum('bdnhqk,bdnkhe->bdnqhe', p, vb)

    def from_sub(t):
        t = t.reshape((b, dilation, n_pad) + t.shape[4:])[:, :, :n_sub]
        return jnp.swapaxes(t, 1, 2).reshape((b, S) + t.shape[3:])

    return (from_sub(num), from_sub(jnp.swapaxes(mx[..., 0], 3, 4)), from_sub(jnp.swapaxes(den, 3, 4)))


def dilated_attention(parts, bias_table):
    b, S = parts[0].shape[0], parts[0].shape[1]
    nums, mxs, dens = [], [], []
    for gi, (window, dilation) in enumerate(DIL_PATTERNS):
        q, k, v = [t.reshape(b, S, DIL_HEADS, DIL_DH) for t in parts[3 * gi:3 * gi + 3]]
        num, mx, den = dilated_group(q, k, v, window, dilation,
                                     bias_table[:, gi * DIL_HEADS:(gi + 1) * DIL_HEADS])
        nums.append(num)
        mxs.append(mx)
        dens.append(den)
    num, mx, den = jnp.stack(nums), jnp.stack(mxs), jnp.stack(dens)
    wgt = jnp.exp(mx - jnp.max(mx, axis=0, keepdims=True))
    o = jnp.sum(wgt[..., None] * num, axis=0) / jnp.sum(wgt * den, axis=0)[..., None]
    return o.reshape(b, S, DIL_HEADS * DIL_DH).astype(parts[0].dtype)


def setup_inputs(seed: int = 0) -> dict:
    key = jax.random.key(seed)
    ks = jax.random.split(key, 16)

    def nrm(k, shape, scale):
        return jax.random.normal(k, shape, jnp.float32) * scale

    return {
        "x": nrm(ks[0], (BATCH, SEQ, D_MODEL), 1.0),
        "w_in": nrm(ks[1], (DEPTH, D_MODEL, IN_WIDTH), D_MODEL ** -0.5),
        "w_branch_ret": nrm(ks[2], (DEPTH, RET_HEADS * RET_DV, D_MODEL), (RET_HEADS * RET_DV) ** -0.5),
        "w_branch_diff": nrm(ks[3], (DEPTH, DIFF_HEADS * 2 * DIFF_DH, D_MODEL), (DIFF_HEADS * 2 * DIFF_DH) ** -0.5),
        "w_branch_dil": nrm(ks[4], (DEPTH, DIL_HEADS * DIL_DH, D_MODEL), (DIL_HEADS * DIL_DH) ** -0.5),
        "w_out": nrm(ks[5], (DEPTH, D_MODEL, D_MODEL), D_MODEL ** -0.5),
        "norm_mix": 1.0 + nrm(ks[6], (DEPTH, D_MODEL), 0.02),
        "norm_ffn": 1.0 + nrm(ks[7], (DEPTH, D_MODEL), 0.02),
        "ret_gn_gain": 1.0 + nrm(ks[8], (DEPTH, RET_HEADS * RET_DV), 0.02),
        "diff_lambda": nrm(ks[9], (DEPTH, 4, DIFF_DH), 0.1),
        "diff_subln_gain": 1.0 + nrm(ks[10], (DEPTH, 2 * DIFF_DH), 0.02),
        "rel_bias": nrm(ks[11], (REL_BUCKETS, N_BIAS_HEADS), 0.2),
        "w_ffn_gate": nrm(ks[12], (DEPTH, D_MODEL, FFN_HIDDEN), D_MODEL ** -0.5),
        "w_ffn_up": nrm(ks[13], (DEPTH, D_MODEL, FFN_HIDDEN), D_MODEL ** -0.5),
        "w_ffn_down": nrm(ks[14], (DEPTH, FFN_HIDDEN, D_MODEL), FFN_HIDDEN ** -0.5),
        "norm_final": 1.0 + nrm(ks[15], (D_MODEL,), 0.02),
    }


def reference(x, w_in, w_branch_ret, w_branch_diff, w_branch_dil, w_out, norm_mix, norm_ffn,
              ret_gn_gain, diff_lambda, diff_subln_gain, rel_bias, w_ffn_gate, w_ffn_up, w_ffn_down,
              norm_final):
    b, S, _ = x.shape
    cuts = np.cumsum(IN_SPLITS)[:-1].tolist()
    for l in range(DEPTH):
        h = rmsnorm(x, norm_mix[l])
        parts = jnp.split(h @ w_in[l], cuts, axis=-1)
        rq, rk, rv, rg = parts[0:4]
        dq, dkk, dvv = parts[4:7]
        dil_parts = parts[7:7 + 3 * N_DIL]
        gates = jax.nn.sigmoid(parts[-1].astype(jnp.float32)).reshape(b, S, N_BRANCHES, D_MODEL).astype(x.dtype)
        y_ret = retention(rq.reshape(b, S, RET_HEADS, RET_DK), rk.reshape(b, S, RET_HEADS, RET_DK),
                          rv, rg, ret_gn_gain[l])
        lam_init = 0.8 - 0.6 * math.exp(-0.3 * l)
        y_diff = diff_attention(dq.reshape(b, S, DIFF_HEADS, 2, DIFF_DH), dkk.reshape(b, S, DIFF_HEADS, 2, DIFF_DH),
                                dvv.reshape(b, S, DIFF_HEADS, 2 * DIFF_DH), diff_lambda[l], lam_init,
                                diff_subln_gain[l], rel_bias[:, :DIFF_HEADS])
        y_dil = dilated_attention(dil_parts, rel_bias[:, DIFF_HEADS:])
        merged = (gates[:, :, 0] * (y_ret @ w_branch_ret[l])
                  + gates[:, :, 1] * (y_diff @ w_branch_diff[l])
                  + gates[:, :, 2] * (y_dil @ w_branch_dil[l]))
        x = x + merged @ w_out[l]
        h = rmsnorm(x, norm_ffn[l])
        x = x + (jax.nn.silu(h @ w_ffn_gate[l]) * (h @ w_ffn_up[l])) @ w_ffn_down[l]
    return rmsnorm(x, norm_final)
```

```python
import math
import contextlib
import numpy as np
import ml_dtypes
import concourse.bass as bass
import concourse.mybir as mybir
from concourse.bass_utils import run_bass_kernel_spmd

F32 = mybir.dt.float32
BF16 = mybir.dt.bfloat16
AF = mybir.ActivationFunctionType
ALU = mybir.AluOpType
ENGS = ['pe', 'act', 'dve', 'pool', 'sp']

D = 1024
S = 2048
NCORES = 8
SEQ_PER_CORE = 2
DEPTH = 2
IN_WIDTH = 13824
FFN = 2816
MASKV = -30000.0
RMS_EPS = 1e-6
GN_EPS = 1e-5
DIFF_EPS = 1e-5
DIL = ((128, 1), (512, 4), (2048, 16))
import os as _os
DILG = [int(v) for v in _os.environ.get('DILG', '0,1,2').split(',')]


class Prog:
    def __init__(self, nc, n_dma_sems=24, same_eng_sync=True):
        self.nc = nc
        self.ops = {e: [] for e in ENGS}
        self.res = {}
        self.ndma = 0
        self.ndmaq = {}
        self.ND = n_dma_sems
        self.same = same_eng_sync
        self.out_tokens = []
        self.last_compute = {}
        self.dma_since_fence = []

    def _deps(self, reads, writes):
        deps = set()
        for r in reads:
            st = self.res.get(r)
            if st and st['w'] is not None:
                deps.add(st['w'])
        for w in writes:
            st = self.res.get(w)
            if st:
                if st['w'] is not None:
                    deps.add(st['w'])
                deps.update(st['r'])
        return deps

    def _commit(self, tok, reads, writes):
        for r in reads:
            st = self.res.setdefault(r, {'w': None, 'r': set()})
            st['r'] = {t for t in st['r'] if not (t[0] == tok[0] and t[0] != 'dma')} | {tok}
        for w in writes:
            self.res[w] = {'w': tok, 'r': set()}

    def op(self, eng, fn, reads=(), writes=()):
        deps = self._deps(reads, writes)
        idx = len(self.ops[eng])
        tok = (eng, idx)
        self.ops[eng].append(dict(fn=fn, deps=deps, dma=None))
        self._commit(tok, reads, writes)
        self.last_compute[eng] = tok
        return tok

    def dma(self, queue, fn, reads=(), writes=(), is_output=False):
        deps = self._deps(reads, writes)
        k = self.ndmaq.get(queue, 0)
        self.ndmaq[queue] = k + 1
        self.ndma += 1
        if k >= self.ND:
            deps.add(('dma', (queue, k - self.ND)))
        self.ops[queue].append(dict(fn=fn, deps=deps, dma=(queue, k)))
        tok = ('dma', (queue, k))
        self._commit(tok, reads, writes)
        self.dma_since_fence.append(tok)
        if is_output:
            self.out_tokens.append(tok)
        return tok

    def fence(self):
        deps = set(self.last_compute.values())
        for q in ('sp', 'pool'):
            deps |= set([t for t in self.dma_since_fence if t[1][0] == q][-self.ND:])
        self.dma_since_fence = []
        for e in ENGS:
            self.ops[e].append(dict(fn=None, deps=set(deps), dma=None))

    def finish(self):
        self.ops['sp'].append(dict(fn=None, deps=set(self.out_tokens[-self.ND:]), dma=None))

    def emit(self):
        nc = self.nc
        same = self.same
        ND = self.ND
        need = {e: set() for e in ENGS}
        for e in ENGS:
            for o in self.ops[e]:
                for d in o['deps']:
                    if d[0] == 'dma':
                        continue
                    if d[0] == e and (e == 'pe' or not same):
                        continue
                    need[d[0]].add(d[1])
        tick = {e: {} for e in ENGS}
        for e in ENGS:
            c = 0
            for i in range(len(self.ops[e])):
                if i in need[e]:
                    c += 1
                    tick[e][i] = c
        self.stats = {e: [len(self.ops[e]), len(need[e])] for e in ENGS}
        with contextlib.ExitStack() as st:
            esem = {e: st.enter_context(nc.semaphore("s_" + e)) for e in ENGS}
            dsem = {q: [st.enter_context(nc.semaphore("d_%s_%d" % (q, i))) for i in range(ND)] for q in ('sp', 'pool')}
            block = st.enter_context(nc.Block())

            def mk(e):
                def body(eng):
                    waited = {}
                    nw = 0
                    for i, o in enumerate(self.ops[e]):
                        for d in sorted(o['deps']):
                            if d[0] == 'dma':
                                dq, dk = d[1]
                                key = ('dma', dq, dk % ND)
                                v = 16 * (dk // ND + 1)
                                sem = dsem[dq][dk % ND]
                            else:
                                if d[0] == e and (e == 'pe' or not same):
                                    continue
                                key = d[0]
                                v = tick[d[0]][d[1]]
                                sem = esem[d[0]]
                            if waited.get(key, 0) >= v:
                                continue
                            waited[key] = v
                            eng.wait_ge(sem, v)
                            nw += 1
                        if o['fn'] is None:
                            continue
                        ins = o['fn'](eng)
                        if o['dma'] is not None:
                            ins.then_inc(dsem[o['dma'][0]][o['dma'][1] % ND], 16)
                        elif i in need[e]:
                            ins.then_inc(esem[e], 1)
                    self.stats[e].append(nw)
                return body

            block.tensor(mk('pe'))
            block.scalar(mk('act'))
            block.vector(mk('dve'))
            block.gpsimd(mk('pool'))
            block.sync(mk('sp'))


def _rel_bucket(n):
    n = np.maximum(n, 0)
    exact = 16
    lr = np.log(np.maximum(n, exact).astype(np.float32) / np.float32(exact)) / np.float32(math.log(128 / 16))
    large = np.minimum(exact + (lr * np.float32(16)).astype(np.int32), 31)
    return np.where(n < exact, n, large)


def _host_consts(rel_bias):
    c = {}
    ident = np.eye(128, dtype=np.float32)
    ones = np.ones((128, 128), np.float32)
    perm = np.zeros((128, 128), np.float32)
    for m in range(128):
        perm[(m + 64) % 128, m] = 1.0
    c["cst_bf"] = np.stack([ident, ones, perm], axis=1).astype(ml_dtypes.bfloat16)
    half = 64
    inv = (np.float32(10000.0) ** (-np.arange(half, dtype=np.float32) / np.float32(half))).astype(np.float32)
    pos = np.arange(S, dtype=np.float32)
    ang = (pos[:, None] * inv[None, :]).astype(np.float32)
    cos = np.cos(ang).astype(np.float32).T
    sin = np.sin(ang).astype(np.float32).T
    c["rot"] = np.stack([np.concatenate([cos, cos], 0), np.concatenate([-sin, sin], 0)], 0).astype(np.float32)
    rc = np.zeros((4, 128, 644), np.float32)
    i = np.arange(128, dtype=np.float64)
    for h in range(4):
        lg = math.log1p(-2.0 ** (-5.0 - h))
        rel = i[None, :] - i[:, None]
        rc[h, :, 0:128] = np.where(rel >= 0, np.exp(lg * np.maximum(rel, 0.0)), 0.0)
        rc[h, :, 128:640] = np.tile(np.exp((i + 1.0) * lg)[None, :], (128, 4))
        rc[h, :, 640] = np.exp((127.0 - i) * lg)
        rc[h, :, 641] = math.exp(128.0 * lg)
    c["rconst"] = rc
    rb = np.asarray(rel_bias, np.float32)
    kj = np.arange(128)[:, None]
    qi = np.arange(256)[None, :]
    dist = qi - kj
    db = np.zeros((8, 128, 256), np.float32)
    bk = _rel_bucket(dist)
    for h in range(8):
        db[h] = np.where(dist >= 0, rb[bk, h], np.float32(MASKV))
    c["dbias"] = db
    c["cbias"] = np.ascontiguousarray(np.broadcast_to(rb[31:32, :], (128, 20))).astype(np.float32)
    lb = np.zeros((12, 128, 256), np.float32)
    for g, (win, dil) in enumerate(DIL):
        m = qi - kj
        valid = (m >= 0) & (m <= win // dil)
        bk = _rel_bucket(m * dil)
        for hs in range(4):
            lb[g * 4 + hs] = np.where(valid, rb[bk, 8 + g * 4 + hs], np.float32(MASKV))
    c["lbias"] = lb
    return c


def build_program(n_seq=SEQ_PER_CORE, layers=(0, 1), phases=("n1", "ret", "diff", "dil", "merge", "ffn", "final"),
                  dbg=False, ret_heads=range(4), diff_heads=range(8), dil_slots=range(4), same_eng_sync=True):
    nc = bass.Bass("TRN2", target_bir_lowering=False)
    P = Prog(nc, same_eng_sync=same_eng_sync)

    def din(name, shape, dt=F32):
        return nc.dram_tensor(name, list(shape), dt, kind="ExternalInput")

    x_d = din("x", [n_seq, S, D])
    w_in = din("w_in", [DEPTH, D, IN_WIDTH])
    w_bret = din("w_branch_ret", [DEPTH, 1024, D])
    w_bdiff = din("w_branch_diff", [DEPTH, 1024, D])
    w_bdil = din("w_branch_dil", [DEPTH, 512, D])
    w_out = din("w_out", [DEPTH, D, D])
    norm_mix = din("norm_mix", [DEPTH, D])
    norm_ffn = din("norm_ffn", [DEPTH, D])
    gn_gain = din("ret_gn_gain", [DEPTH, 1024])
    dlam = din("diff_lambda", [DEPTH, 4, 64])
    subln = din("diff_subln_gain", [DEPTH, 128])
    w_g = din("w_ffn_gate", [DEPTH, D, FFN])
    w_u = din("w_ffn_up", [DEPTH, D, FFN])
    w_d = din("w_ffn_down", [DEPTH, FFN, D])
    norm_final = din("norm_final", [D])
    cst_bf_d = din("cst_bf", [128, 3, 128], BF16)
    rot_d = din("rot", [2, 128, S])
    rconst_d = din("rconst", [4, 128, 644])
    dbias_d = din("dbias", [8, 128, 256])
    cbias_d = din("cbias", [128, 20])
    lbias_d = din("lbias", [12, 128, 256])
    okind = "ExternalOutput" if dbg else "Internal"
    out_d = nc.dram_tensor("out", [n_seq, S, D], F32, kind="ExternalOutput")
    xs_d = nc.dram_tensor("xs", [n_seq, S, D], F32, kind=okind)
    yb_d = nc.dram_tensor("ybuf", [2560, S], BF16, kind=okind)

    st = contextlib.ExitStack()
    with st:
        def sb(name, shape, dt):
            return st.enter_context(nc.sbuf_tensor(name, shape, dt))

        cst = sb("cst", [128, 3, 128], BF16)
        ident = cst[:, 0, :]
        ones = cst[:, 1, :]
        perm = cst[:, 2, :]
        hT = sb("hT", [128, 8, S], BF16)
        NW = 4
        wring = [sb("wr%d" % i, [128, 8, 512], BF16) for i in range(NW)]
        cbias = sb("cbias_sb", [128, 20], F32)
        smalls = sb("smalls", [128, 64], F32)
        ARENA = 100 * 1024
        arena_bf = sb("arena", [128, ARENA // 2], BF16)
        arena_f = arena_bf.bitcast(F32)
        psum = [st.enter_context(nc.psum_tensor("ps%d" % i, [128, 512], F32)) for i in range(8)]
        psum_bf = [p.bitcast(BF16) for p in psum]

        class Arena:
            def __init__(self):
                self.off = 0

            def reset(self, off=0):
                self.off = off

            def f32(self, n):
                o = self.off
                self.off += ((n * 4 + 63) // 64) * 64
                assert self.off <= ARENA, ("arena overflow", self.off)
                return arena_f[:, o // 4:o // 4 + n]

            def bf(self, n):
                o = self.off
                self.off += ((n * 2 + 63) // 64) * 64
                assert self.off <= ARENA, ("arena overflow", self.off)
                return arena_bf[:, o // 2:o // 2 + n]

        A = Arena()
        wstate = {'n': 0}

        def PS(b):
            return ('ps', b)

        def wload(pieces):
            slot = wstate['n'] % NW
            wstate['n'] += 1
            key = ('w', slot)
            offs = []
            c0 = 0
            for src in pieces:
                rows, ncols = src.shape
                nch = rows // 128
                v = src.rearrange("(c p) n -> p c n", p=128)
                P.dma('pool', lambda e, v=v, slot=slot, nch=nch, c0=c0, ncols=ncols:
                      e.dma_start(out=wring[slot][:, 0:nch, c0:c0 + ncols], in_=v), writes=[(key, len(offs))])
                offs.append(c0)
                c0 += ncols
            assert c0 <= 512
            keys = [(key, i) for i in range(len(offs))]
            return wring[slot], keys, offs

        epsg = smalls[:, 60:61]
        epsd = smalls[:, 61:62]
        P.op('dve', lambda e: e.memset(smalls[:, 60:61], GN_EPS), writes=['epsv'])
        P.op('dve', lambda e: e.memset(smalls[:, 61:62], DIFF_EPS), writes=['epsv'])
        P.dma('sp', lambda e: e.dma_start(out=cst[:, :, :], in_=cst_bf_d.ap()), writes=['cst'])
        P.dma('sp', lambda e: e.dma_start(out=cbias[:, :], in_=cbias_d.ap()), writes=['cbias'])

        def bcast_row(t, offset, n):
            return bass.AP(t, offset, [[0, 128], [1, n]])

        def col_vec(t, offset, n=128):
            return bass.AP(t, offset, [[1, n], [1, 1]])

        def rms_stats(xtile, xkey, sq, idx, eps):
            ss = smalls[:, idx * 3:idx * 3 + 1]
            sd = smalls[:, idx * 3 + 1:idx * 3 + 2]
            rs = smalls[:, idx * 3 + 2:idx * 3 + 3]
            k = ('st', idx)
            P.op('act', lambda e: e.activation(out=sq, in_=xtile, func=AF.Square, accum_out=ss),
                 reads=[xkey], writes=['sq', k])
            P.op('act', lambda e: e.activation(out=sd, in_=ss, func=AF.Sqrt, scale=1.0 / D, bias=eps),
                 reads=[], writes=[k])
            P.op('dve', lambda e: e.reciprocal(out=rs, in_=sd), reads=[], writes=[k])
            return rs, k

        def norm_phase_hT(xsrc, gain_t, gain_off):
            A.reset()
            xt = [A.f32(D) for _ in range(3)]
            sq = A.f32(D)
            hn = [A.bf(D) for _ in range(2)]
            gbc = A.f32(D)
            P.dma('sp', lambda e: e.dma_start(out=gbc, in_=bcast_row(gain_t, gain_off, D)), writes=['gbc'])
            for t in range(16):
                xb = xt[t % 3]
                xk = ('xt', t % 3)
                P.dma('sp', lambda e, xb=xb, t=t: e.dma_start(out=xb, in_=xsrc[t * 128:(t + 1) * 128, :]), writes=[xk])
                rs, k = rms_stats(xb, xk, sq, t % 8, RMS_EPS)
                hb = hn[t % 2]
                hk = ('hn', t % 2)
                P.op('dve', lambda e, hb=hb, xb=xb, rs=rs: e.scalar_tensor_tensor(
                    out=hb, in0=xb, scalar=rs, in1=gbc, op0=ALU.mult, op1=ALU.mult),
                    reads=[xk, k, 'gbc'], writes=[hk])
                b = t % 2
                pv = psum_bf[b][:, 0:1024].rearrange("p (c n) -> p c n", c=8)
                for c in range(8):
                    P.op('pe', lambda e, pv=pv, hb=hb, c=c: e.transpose(out=pv[:, c, :], in_=hb[:, c * 128:(c + 1) * 128],
                                                                       identity=ident), reads=[hk, 'cst'], writes=[PS(b)])
                P.op('act', lambda e, pv=pv, t=t: e.activation(out=hT[:, :, t * 128:(t + 1) * 128], in_=pv, func=AF.Copy),
                     writes=[PS(b), ('hT', t // 4)])

        HT4 = [('hT', j) for j in range(4)]

        def proj_fm(wt, wkeys_i, col0, ncols, j, bank, evac):
            for c in range(8):
                P.op('pe', lambda e, c=c: e.matmul(psum[bank][0:ncols, :], lhsT=wt[:, c, col0:col0 + ncols],
                                                   rhs=hT[:, c, j * 512:(j + 1) * 512], start=(c == 0), stop=(c == 7)),
                     reads=[wkeys_i, ('hT', j)], writes=[PS(bank)])
            evac(psum[bank][0:ncols, :], PS(bank))

        def retention_phase(l):
            A.reset()
            cosT = A.f32(S)
            sinT = A.f32(S)
            rcs = [A.f32(644) for _ in range(2)]
            gnv = A.f32(8)
            sgT = A.bf(2 * S).rearrange("p (a n) -> p a n", a=2)
            Qp = A.bf(S)
            Qpp = A.bf(S)
            Kp = A.bf(S)
            raw = [A.bf(512) for _ in range(2)]
            Kpp = A.bf(16 * 128).rearrange("p (n d) -> p n d", n=16)
            V = A.bf(16 * 256).rearrange("p (n e) -> p n e", n=16)
            Sbf = A.bf(16 * 256).rearrange("p (n e) -> p n e", n=16)
            Sst2 = [A.f32(256) for _ in range(2)]
            PT = [A.bf(128) for _ in range(4)]
            t1 = [A.f32(512) for _ in range(2)]
            t2 = [A.f32(512) for _ in range(2)]
            ysq = [A.bf(2 * 512).rearrange("p (a n) -> p a n", a=2) for _ in range(2)]
            ybf = [A.bf(2 * 512).rearrange("p (a n) -> p a n", a=2) for _ in range(2)]
            mean = A.f32(512)
            var = A.f32(512)
            msq = A.f32(512)
            tt = [A.f32(512) for _ in range(2)]
            yout = [A.bf(2 * 512).rearrange("p (a n) -> p a n", a=2) for _ in range(2)]

            P.dma('sp', lambda e: e.dma_start(out=cosT, in_=rot_d.ap()[0]), writes=['cosT'])
            P.dma('sp', lambda e: e.dma_start(out=sinT, in_=rot_d.ap()[1]), writes=['sinT'])
            P.dma('sp', lambda e: e.dma_start(out=gnv.rearrange("p (c o) -> p c o", o=1),
                                              in_=bass.AP(gn_gain, l * 1024, [[1, 128], [128, 8], [1, 1]]),
                                              allow_slow_non_contiguous=True), writes=['gnv'])
            wl = w_in.ap()[l]
            for h in ret_heads:
                rc = rcs[h % 2]
                rck = ('rc', h % 2)
                P.dma('sp', lambda e, rc=rc, h=h: e.dma_start(out=rc, in_=rconst_d.ap()[h]), writes=[rck])
                decT = rc[:, 0:128]
                qdec = rc[:, 128:640]
                kdec = rc[:, 640:641]
                gam = rc[:, 641:642]
                wA, kA, oA = wload([wl[:, h * 128:(h + 1) * 128], wl[:, 512 + h * 128:512 + (h + 1) * 128],
                                    wl[:, 1024 + h * 256:1024 + (h + 1) * 256]])
                wB, kB, oB = wload([wl[:, 2048 + h * 256:2048 + (h + 1) * 256]])
                tiles = [(w_, j) for w_ in ("q", "k") for j in range(4)]

                def rot_proj(i, wA=wA, kA=kA, oA=oA):
                    which, j = tiles[i]
                    bank = i % 3
                    rb_ = raw[i % 2]
                    rk = ('raw', i % 2)
                    scale = 1.0 if which == "q" else 128.0 ** -0.5

                    def evac(ps_ap, psk):
                        P.op('act', lambda e: e.activation(out=rb_, in_=ps_ap, func=AF.Copy, scale=scale), writes=[psk, rk])
                    wi = 0 if which == "q" else 1
                    proj_fm(wA, kA[wi], oA[wi], 128, j, bank, evac)

                def rot_post(i, qdec=qdec, rck=rck):
                    which, j = tiles[i]
                    swb = 3 + i % 2
                    rb_ = raw[i % 2]
                    rk = ('raw', i % 2)
                    a1 = t1[i % 2]
                    a2 = t2[i % 2]
                    tk = ('t12', i % 2)
                    sl = slice(j * 512, (j + 1) * 512)
                    P.op('pe', lambda e: e.matmul(psum[swb][:, :], lhsT=perm, rhs=rb_, start=True, stop=True),
                         reads=[rk, 'cst'], writes=[PS(swb)])
                    P.op('dve', lambda e: e.tensor_tensor(out=a1, in0=rb_, in1=cosT[:, sl], op=ALU.mult),
                         reads=[rk, 'cosT'], writes=[tk])
                    P.op('dve', lambda e: e.tensor_tensor(out=a2, in0=psum[swb][:, :], in1=sinT[:, sl], op=ALU.mult),
                         reads=['sinT'], writes=[PS(swb), (tk, 2)])
                    P.op('dve', lambda e: e.tensor_tensor(out=a1, in0=a1, in1=a2, op=ALU.add),
                         reads=[(tk, 2)], writes=[tk])
                    if which == "q":
                        P.op('act', lambda e: e.activation(out=Qp[:, sl], in_=a1, func=AF.Copy), reads=[tk], writes=[('Qp', j)])
                        P.op('dve', lambda e: e.tensor_tensor(out=Qpp[:, sl], in0=a1, in1=qdec, op=ALU.mult),
                             reads=[tk, rck], writes=[('Qpp', j)])
                    else:
                        P.op('act', lambda e: e.activation(out=Kp[:, sl], in_=a1, func=AF.Copy), reads=[tk], writes=[('Kp', j)])

                rot_proj(0)
                for i in range(8):
                    if i + 1 < 8:
                        rot_proj(i + 1)
                    rot_post(i)
                for t in range(16):
                    bank = t % 3
                    for c in range(8):
                        P.op('pe', lambda e, c=c, t=t, bank=bank, wA=wA, oA=oA: e.matmul(
                            psum[bank][:, 0:256], lhsT=hT[:, c, t * 128:(t + 1) * 128],
                            rhs=wA[:, c, oA[2]:oA[2] + 256], start=(c == 0), stop=(c == 7)),
                            reads=[kA[2], ('hT', t // 4)], writes=[PS(bank)])
                    P.op('act', lambda e, t=t, bank=bank: e.activation(out=V[:, t, :], in_=psum[bank][:, 0:256], func=AF.Copy),
                         writes=[PS(bank), ('V', t)])
                for half in range(2):
                    bank = 5 + half
                    pv = psum_bf[bank][:, 0:1024].rearrange("p (n d) -> p n d", n=8)
                    for n8 in range(8):
                        n = half * 8 + n8
                        P.op('pe', lambda e, pv=pv, n8=n8, n=n: e.transpose(out=pv[:, n8, :], in_=Kp[:, n * 128:(n + 1) * 128], identity=ident),
                             reads=[('Kp', n // 4), 'cst'], writes=[PS(bank)])
                    P.op('dve', lambda e, pv=pv, half=half, kdec=kdec: e.tensor_scalar(
                        out=Kpp[:, half * 8:(half + 1) * 8, :], in0=pv, scalar1=kdec, scalar2=None, op0=ALU.mult),
                        reads=[rck], writes=[PS(bank), ('Kpp', half)])

                def kv_step(n, gam=gam, rck=rck):
                    bank = 3 + n % 2
                    snew = Sst2[(n + 1) % 2]
                    sold = Sst2[n % 2]
                    P.op('pe', lambda e: e.matmul(psum[bank][:, 0:256], lhsT=Kpp[:, n, :], rhs=V[:, n, :], start=True, stop=True),
                         reads=[('Kpp', n // 8), ('V', n)], writes=[PS(bank)])
                    if n == 0:
                        P.op('dve', lambda e: e.tensor_copy(out=snew, in_=psum[bank][:, 0:256]), writes=[PS(bank), ('Sst', (n + 1) % 2)])
                    else:
                        P.op('dve', lambda e: e.scalar_tensor_tensor(
                            out=snew, in0=sold, scalar=gam, in1=psum[bank][:, 0:256], op0=ALU.mult, op1=ALU.add),
                            reads=[rck, ('Sst', n % 2)], writes=[PS(bank), ('Sst', (n + 1) % 2)])
                    P.op('act', lambda e: e.activation(out=Sbf[:, n + 1, :], in_=snew, func=AF.Copy),
                         reads=[('Sst', (n + 1) % 2)], writes=[('Sbf', n + 1)])

                gi_ = 0
                for a in range(2):
                    for j in range(4):
                        bank = gi_ % 3

                        def evac(ps_ap, psk, a=a, j=j):
                            P.op('act', lambda e: e.activation(out=sgT[:, a, j * 512:(j + 1) * 512], in_=ps_ap, func=AF.Silu),
                                 writes=[psk, ('sgT', a, j)])
                        proj_fm(wB, kB[0], oB[0] + a * 128, 128, j, bank, evac)
                        for n in (2 * gi_, 2 * gi_ + 1):
                            if n < 15:
                                kv_step(n)
                        gi_ += 1
                def ybanks(T):
                    return (6, 7) if T % 2 == 0 else (2, 3)

                def stA(n, decT=decT, rck=rck):
                    sb_ = 4 + n % 2
                    pt = PT[n % 4]
                    ptk = ('PT', n % 4)
                    csl = slice(n * 128, (n + 1) * 128)
                    P.op('pe', lambda e: e.matmul(psum[sb_][:, 0:128], lhsT=Kp[:, csl], rhs=Qp[:, csl], start=True, stop=True),
                         reads=[('Kp', n // 4), ('Qp', n // 4)], writes=[PS(sb_)])
                    P.op('dve', lambda e: e.tensor_tensor(out=pt, in0=psum[sb_][:, 0:128], in1=decT, op=ALU.mult),
                         reads=[rck], writes=[PS(sb_), ptk])

                def stB(n):
                    T, nn = n // 4, n % 4
                    yb = ybanks(T)
                    pt = PT[n % 4]
                    ptk = ('PT', n % 4)
                    csl = slice(n * 128, (n + 1) * 128)
                    osl = slice(nn * 128, (nn + 1) * 128)
                    for a in range(2):
                        P.op('pe', lambda e, a=a: e.matmul(
                            psum[yb[a]][:, osl], lhsT=V[:, n, a * 128:(a + 1) * 128], rhs=pt, start=True, stop=(n == 0)),
                            reads=[('V', n), ptk], writes=[PS(yb[a])])
                        if n > 0:
                            P.op('pe', lambda e, a=a: e.matmul(
                                psum[yb[a]][:, osl], lhsT=Sbf[:, n, a * 128:(a + 1) * 128], rhs=Qpp[:, csl], start=False, stop=True),
                                reads=[('Sbf', n), ('Qpp', n // 4)], writes=[PS(yb[a])])

                def g1(T):
                    yb = ybanks(T)
                    ysq_ = ysq[T % 2]
                    ybf_ = ybf[T % 2]
                    yk = ('ysq', T % 2)
                    bk_ = ('ybf', T % 2)
                    for a in range(2):
                        P.op('act', lambda e, a=a: e.activation(out=ysq_[:, a, :], in_=psum[yb[a]][:, :], func=AF.Square),
                             writes=[PS(yb[a]), (yk, a)])
                        P.op('dve', lambda e, a=a: e.tensor_copy(out=ybf_[:, a, :], in_=psum[yb[a]][:, :]),
                             writes=[PS(yb[a]), (bk_, a)])

                def g2_steps(T, h=h):
                    ysq_ = ysq[T % 2]
                    ybf_ = ybf[T % 2]
                    yk = ('ysq', T % 2)
                    bk_ = ('ybf', T % 2)
                    tsl = slice(T * 512, (T + 1) * 512)
                    yo = yout[T % 2]
                    yok = ('yout', T % 2)

                    def s1():
                        for a in range(2):
                            P.op('pe', lambda e, a=a: e.matmul(psum[0][:, :], lhsT=ones, rhs=ybf_[:, a, :], start=(a == 0), stop=(a == 1)),
                                 reads=[(bk_, a), 'cst'], writes=[PS(0)])
                        for a in range(2):
                            P.op('pe', lambda e, a=a: e.matmul(psum[1][:, :], lhsT=ones, rhs=ysq_[:, a, :], start=(a == 0), stop=(a == 1)),
                                 reads=[(yk, a), 'cst'], writes=[PS(1)])
                        P.op('act', lambda e: e.activation(out=mean, in_=psum[0][:, :], func=AF.Copy, scale=1.0 / 256),
                             writes=[PS(0), 'mean'])
                        P.op('act', lambda e: e.activation(out=msq, in_=psum[0][:, :], func=AF.Square, scale=1.0 / 256),
                             writes=[PS(0), 'msq'])
                        P.op('dve', lambda e: e.scalar_tensor_tensor(out=var, in0=psum[1][:, :], scalar=1.0 / 256, in1=msq,
                                                                     op0=ALU.mult, op1=ALU.subtract),
                             reads=['msq'], writes=[PS(1), 'var'])

                    def s2():
                        P.op('act', lambda e: e.activation(out=var, in_=var, func=AF.Ln, bias=epsg, scale=1.0), reads=['epsv'], writes=['var'])
                        P.op('act', lambda e: e.activation(out=var, in_=var, func=AF.Exp, scale=-0.5), writes=['var'])

                    def mk_sub(a):
                        def f():
                            P.op('dve', lambda e: e.tensor_tensor(out=tt[a], in0=ybf_[:, a, :], in1=mean, op=ALU.subtract),
                                 reads=[(bk_, a), 'mean'], writes=[('tt', a)])
                        return f

                    def mk_mul(a):
                        def f():
                            P.op('dve', lambda e: e.tensor_tensor(out=tt[a], in0=tt[a], in1=var, op=ALU.mult),
                                 reads=['var'], writes=[('tt', a)])
                        return f

                    def mk_out(a):
                        def f():
                            P.op('dve', lambda e: e.scalar_tensor_tensor(
                                out=yo[:, a, :], in0=tt[a], scalar=gnv[:, h * 2 + a:h * 2 + a + 1], in1=sgT[:, a, tsl], op0=ALU.mult, op1=ALU.mult),
                                reads=[('tt', a), 'gnv', ('sgT', a, T)], writes=[(yok, a)])
                        return f

                    def s_dma():
                        dst = yb_d.ap()[h * 256:(h + 1) * 256, tsl].rearrange("(a p) n -> p a n", p=128)
                        P.dma('sp', lambda e: e.dma_start(out=dst, in_=yo), reads=[(yok, 0), (yok, 1)],
                              writes=[('ybuf', h * 2, T), ('ybuf', h * 2 + 1, T)])
                    return [s1, s2, mk_sub(0), mk_mul(0), mk_out(0), mk_sub(1), mk_mul(1), mk_out(1), s_dma]

                pend = []
                stA(0)
                for n in range(16):
                    if n + 1 < 16:
                        stA(n + 1)
                    stB(n)
                    for _ in range(3):
                        if pend:
                            pend.pop(0)()
                    if n % 4 == 3:
                        g1(n // 4)
                        pend += g2_steps(n // 4)
                for st_ in pend:
                    st_()

        def diff_phase(l):
            A.reset()
            lam_init = 0.8 - 0.6 * math.exp(-0.3 * l)
            lp = A.f32(256)
            ltmp = A.f32(64)
            lsc = A.f32(8)
            sgn = A.f32(2)
            QTm = [A.bf(S) for _ in range(2)]
            KT = A.bf(S)
            V = A.bf(16 * 128).rearrange("p (n e) -> p n e", n=16)
            btile = [A.f32(256) for _ in range(2)]
            tmpb = [A.f32(256) for _ in range(2)]
            Pm = [A.bf(512) for _ in range(4)]
            P.op('dve', lambda e: e.memset(QTm[0][64:128, :], 0.0), writes=[('QT', j) for j in range(4)])
            P.op('dve', lambda e: e.memset(QTm[1][0:64, :], 0.0), writes=[('QT', j) for j in range(4)])
            r_ = [[A.f32(512) for _ in range(2)] for _ in range(2)]
            o_ = [[A.f32(512) for _ in range(2)] for _ in range(2)]
            osq_ = [A.bf(512) for _ in range(2)]
            sd_ = [A.f32(512) for _ in range(2)]
            yo_ = [A.bf(512) for _ in range(2)]
            P.dma('sp', lambda e: e.dma_start(out=lp, in_=bcast_row(dlam, l * 256, 256)), writes=['lp'])
            P.dma('sp', lambda e: e.dma_start(out=sgn[:, 0:1], in_=col_vec(subln, l * 128)), writes=['sgn'])
            for i in range(2):
                P.op('dve', lambda e, i=i: e.tensor_tensor(out=ltmp, in0=lp[:, i * 128:i * 128 + 64], in1=lp[:, i * 128 + 64:i * 128 + 128], op=ALU.mult),
                     reads=['lp'], writes=['ltmp'])
                P.op('dve', lambda e, i=i: e.reduce_sum(out=lsc[:, i:i + 1], in_=ltmp, axis=mybir.AxisListType.X), reads=['ltmp'], writes=['lsc'])
                P.op('act', lambda e, i=i: e.activation(out=lsc[:, 2 + i:3 + i], in_=lsc[:, i:i + 1], func=AF.Exp), writes=['lsc'])
            P.op('dve', lambda e: e.tensor_tensor(out=lsc[:, 4:5], in0=lsc[:, 2:3], in1=lsc[:, 3:4], op=ALU.subtract), writes=['lsc'])
            P.op('dve', lambda e: e.tensor_scalar(out=lsc[:, 5:6], in0=lsc[:, 4:5], scalar1=lam_init, scalar2=-1.0, op0=ALU.add, op1=ALU.mult),
                 writes=['lsc'])
            nlam = lsc[:, 5:6]
            P.op('dve', lambda e: e.tensor_scalar(out=sgn[:, 1:2], in0=sgn[:, 0:1], scalar1=1.0 - lam_init, scalar2=None, op0=ALU.mult),
                 reads=[], writes=['sgn'])
            sgv = sgn[:, 1:2]
            wl = w_in.ap()[l]
            for h in diff_heads:
                bt = btile[h % 2]
                btk = ('bt', h % 2)
                P.dma('sp', lambda e, bt=bt, h=h: e.dma_start(out=bt, in_=dbias_d.ap()[h]), writes=[btk])
                cb = cbias[:, h:h + 1]
                wA, kA, oA = wload([wl[:, 3072 + h * 128:3072 + (h + 1) * 128], wl[:, 4096 + h * 128:4096 + (h + 1) * 128],
                                    wl[:, 5120 + h * 128:5120 + (h + 1) * 128]])
                pbanks = (7, 0, 1, 2)
                pc = 0
                for j in range(4):
                    sl = slice(j * 512, (j + 1) * 512)

                    def evq(ps_ap, psk, sl=sl, j=j):
                        P.op('act', lambda e: e.activation(out=QTm[0][0:64, sl], in_=ps_ap[0:64, :], func=AF.Copy, scale=0.125),
                             writes=[psk, ('QT', j)])
                        P.op('dve', lambda e: e.tensor_scalar(out=QTm[1][64:128, sl], in0=ps_ap[64:128, :], scalar1=0.125, scalar2=None, op0=ALU.mult),
                             writes=[psk, ('QT', j)])

                    def evk(ps_ap, psk, sl=sl, j=j):
                        P.op('dve', lambda e: e.tensor_copy(out=KT[:, sl], in_=ps_ap), writes=[psk, ('KT', j)])
                    proj_fm(wA, kA[0], oA[0], 128, j, pbanks[pc % 4], evq)
                    pc += 1
                    proj_fm(wA, kA[1], oA[1], 128, j, pbanks[pc % 4], evk)
                    pc += 1
                for t in range(16):
                    vb = pbanks[pc % 4]
                    pc += 1
                    for c in range(8):
                        P.op('pe', lambda e, c=c, t=t, wA=wA, oA=oA, vb=vb: e.matmul(
                            psum[vb][:, 0:128], lhsT=hT[:, c, t * 128:(t + 1) * 128], rhs=wA[:, c, oA[2]:oA[2] + 128],
                            start=(c == 0), stop=(c == 7)), reads=[kA[2], ('hT', t // 4)], writes=[PS(vb)])
                    if t % 2 == 0:
                        P.op('act', lambda e, t=t, vb=vb: e.activation(out=V[:, t, :], in_=psum[vb][:, 0:128], func=AF.Copy), writes=[PS(vb), ('V', t)])
                    else:
                        P.op('dve', lambda e, t=t, vb=vb: e.tensor_copy(out=V[:, t, :], in_=psum[vb][:, 0:128]), writes=[PS(vb), ('V', t)])
                items = [(Q, j, m) for Q in range(4) for j in range(4 * Q + 4) for m in range(2)]

                def stageA(idx, bt=bt, cb=cb, btk=btk):
                    Q, j, m = items[idx]
                    r = j - 4 * Q
                    q0 = max(r, 0) * 128
                    sbk = idx % 3
                    pm = Pm[idx % 4]
                    pmk = ('Pm', idx % 4)
                    P.op('pe', lambda e: e.matmul(
                        psum[sbk][:, q0:512], lhsT=KT[:, j * 128:(j + 1) * 128], rhs=QTm[m][:, Q * 512 + q0:(Q + 1) * 512],
                        start=True, stop=True), reads=[('KT', j // 4), ('QT', Q)], writes=[PS(sbk)])
                    if r >= -1:
                        if r == -1:
                            s0, s1, b0 = 0, 128, 128
                        else:
                            s0, s1, b0 = q0, min(q0 + 256, 512), 0
                        tb = tmpb[idx % 2]
                        tbk = ('tmpb', idx % 2)
                        w_ = s1 - s0
                        P.op('dve', lambda e: e.tensor_tensor(
                            out=tb[:, 0:w_], in0=psum[sbk][:, s0:s1], in1=bt[:, b0:b0 + w_], op=ALU.add),
                            reads=[btk], writes=[PS(sbk), tbk])
                        P.op('act', lambda e: e.activation(out=pm[:, s0:s1], in_=tb[:, 0:w_], func=AF.Exp),
                             reads=[tbk], writes=[(pmk, 0)])
                        c0 = s1
                    else:
                        c0 = 0
                    if c0 < 512:
                        P.op('act', lambda e: e.activation(out=pm[:, c0:512], in_=psum[sbk][:, c0:512], func=AF.Exp, bias=cb),
                             reads=['cbias'], writes=[PS(sbk), (pmk, 1)])

                def stageB(idx):
                    Q, j, m = items[idx]
                    nj = 4 * Q + 4
                    q0 = max(j - 4 * Q, 0) * 128
                    pm = Pm[idx % 4]
                    pmk = ('Pm', idx % 4)
                    P.op('pe', lambda e: e.matmul(
                        psum[3 + m][:, q0:512], lhsT=V[:, j, :], rhs=pm[:, q0:512], start=(j == 0), stop=(j == nj - 1)),
                        reads=[('V', j), (pmk, 0), (pmk, 1)], writes=[PS(3 + m)])
                    P.op('pe', lambda e: e.matmul(
                        psum[5 + m][:, q0:512], lhsT=ones, rhs=pm[:, q0:512], start=(j == 0), stop=(j == nj - 1)),
                        reads=['cst', (pmk, 0), (pmk, 1)], writes=[PS(5 + m)])

                def fin_steps(Q, h=h):
                    rr = r_[Q % 2]
                    oo = o_[Q % 2]
                    osq = osq_[Q % 2]
                    sd = sd_[Q % 2]
                    qk = Q % 2
                    yo = yo_[Q % 2]
                    yok = ('yo', Q % 2)
                    r0 = 1024 + h * 128
                    for m in range(2):
                        P.op('dve', lambda e, m=m: e.tensor_copy(out=rr[m], in_=psum[5 + m][:, :]), writes=[PS(5 + m), ('r', qk, m)])
                        P.op('dve', lambda e, m=m: e.tensor_copy(out=oo[m], in_=psum[3 + m][:, :]), writes=[PS(3 + m), ('o', qk, m)])
                    steps = []
                    for m in range(2):
                        steps.append(lambda m=m: P.op('act', lambda e: e.activation(out=rr[m], in_=rr[m], func=AF.Ln), writes=[('r', qk, m)]))
                        steps.append(lambda m=m: P.op('act', lambda e: e.activation(out=rr[m], in_=rr[m], func=AF.Exp, scale=-1.0), writes=[('r', qk, m)]))
                        steps.append(lambda m=m: P.op('dve', lambda e: e.tensor_tensor(out=oo[m], in0=oo[m], in1=rr[m], op=ALU.mult),
                                                      reads=[('r', qk, m)], writes=[('o', qk, m)]))

                    def s7():
                        P.op('dve', lambda e: e.scalar_tensor_tensor(out=oo[0], in0=oo[1], scalar=nlam, in1=oo[0], op0=ALU.mult, op1=ALU.add),
                             reads=[('o', qk, 1), 'lsc'], writes=[('o', qk, 0)])
                        P.op('act', lambda e: e.activation(out=osq, in_=oo[0], func=AF.Square), reads=[('o', qk, 0)], writes=[('osq', qk)])

                    def s8():
                        P.op('pe', lambda e: e.matmul(psum[7][:, :], lhsT=ones, rhs=osq, start=True, stop=True), reads=[('osq', qk), 'cst'], writes=[PS(7)])
                        P.op('act', lambda e: e.activation(out=sd, in_=psum[7][:, :], func=AF.Ln, bias=epsd, scale=1.0 / 128), reads=['epsv'],
                             writes=[PS(7), ('sd', qk)])

                    def s9():
                        P.op('act', lambda e: e.activation(out=sd, in_=sd, func=AF.Exp, scale=-0.5), writes=[('sd', qk)])
                        P.op('dve', lambda e: e.scalar_tensor_tensor(out=yo, in0=oo[0], scalar=sgv, in1=sd, op0=ALU.mult, op1=ALU.mult),
                             reads=[('o', qk, 0), ('sd', qk), 'sgn'], writes=[yok])
                        P.dma('sp', lambda e: e.dma_start(out=yb_d.ap()[r0:r0 + 128, Q * 512:(Q + 1) * 512], in_=yo),
                              reads=[yok], writes=[('ybuf', r0 // 128, Q)])
                    steps += [s7, s8, s9]
                    return steps

                LOOK = 2
                n_it = len(items)
                pending = []
                for i in range(min(LOOK, n_it)):
                    stageA(i)
                for i in range(n_it):
                    if i + LOOK < n_it:
                        stageA(i + LOOK)
                    stageB(i)
                    Q, j, m = items[i]
                    if pending:
                        pending.pop(0)()
                    if j == 4 * Q + 3 and m == 1:
                        for st_ in pending:
                            st_()
                        pending = fin_steps(Q)
                for st_ in pending:
                    st_()

        def dil_phase(l):
            A.reset()
            QT = [A.bf(S) for _ in range(2)]
            KT = [A.bf(S) for _ in range(2)]
            Vs = [A.bf(16 * 128).rearrange("p (n e) -> p n e", n=16) for _ in range(2)]
            btile = [A.f32(256) for _ in range(2)]
            tmpb = [A.f32(256) for _ in range(2)]
            Pm = [A.bf(256) for _ in range(4)]
            acc = A.f32(2 * S).rearrange("p (a n) -> p a n", a=2)
            yo = A.bf(S)
            wl = w_in.ap()[l]
            gi = 0
            for hs in dil_slots:
                for g, (win, dil) in enumerate(DIL):
                    if g not in DILG:
                        continue
                    nb = S // dil // 128
                    qt = QT[gi % 2]
                    kt = KT[gi % 2]
                    vs = Vs[gi % 2]
                    bt = btile[gi % 2]
                    gk = gi % 2
                    gi += 1
                    P.dma('sp', lambda e, bt=bt, g=g, hs=hs: e.dma_start(out=bt, in_=lbias_d.ap()[g * 4 + hs]), writes=[('bt', gk)])
                    base = 6144 + g * 1536 + hs * 128
                    wA, kA, oA = wload([wl[:, base:base + 128], wl[:, base + 512:base + 640], wl[:, base + 1024:base + 1152]])
                    for j in range(4):
                        sl = slice(j * 512, (j + 1) * 512)

                        def evq(ps_ap, psk, sl=sl, j=j, qt=qt):
                            P.op('act', lambda e: e.activation(out=qt[:, sl], in_=ps_ap, func=AF.Copy), writes=[psk, ('QT', gk, j)])

                        def evk(ps_ap, psk, sl=sl, j=j, kt=kt):
                            P.op('dve', lambda e: e.tensor_copy(out=kt[:, sl], in_=ps_ap), writes=[psk, ('KT', gk, j)])
                        proj_fm(wA, kA[0], oA[0], 128, j, 0, evq)
                        proj_fm(wA, kA[1], oA[1], 128, j, 1, evk)
                    HTall = [('hT', j) for j in range(4)]
                    for r in range(dil):
                        for jb in range(nb):
                            ti = r * nb + jb
                            t0 = r + dil * jb * 128
                            tsl = slice(t0, t0 + dil * 127 + 1, dil)
                            bank = ti % 2
                            for c in range(8):
                                P.op('pe', lambda e, c=c, tsl=tsl, bank=bank, wA=wA, oA=oA: e.matmul(
                                    psum[bank][:, 0:128], lhsT=hT[:, c, tsl], rhs=wA[:, c, oA[2]:oA[2] + 128],
                                    start=(c == 0), stop=(c == 7)), reads=[kA[2]] + HTall, writes=[PS(bank)])
                            P.op('act', lambda e, ti=ti, bank=bank, vs=vs: e.activation(out=vs[:, ti, :], in_=psum[bank][:, 0:128], func=AF.Copy),
                                 writes=[PS(bank), ('V', gk, ti)])
                    scale = 128.0 ** -0.5
                    QTall = [('QT', gk, j) for j in range(4)]
                    KTall = [('KT', gk, j) for j in range(4)]
                    items = [(r, jb) for r in range(dil) for jb in range(nb)]
                    first_g = (g == DILG[0])

                    def stageA(idx, kt=kt, qt=qt, bt=bt, gk=gk, dil=dil, nb=nb, items=items, QTall=QTall, KTall=KTall):
                        r, jb = items[idx]
                        nq = 256 if jb < nb - 1 else 128
                        t0 = r + dil * jb * 128
                        ksl = slice(t0, t0 + dil * 127 + 1, dil)
                        qsl = slice(t0, t0 + dil * (nq - 1) + 1, dil)
                        sbk = 2 + idx % 3
                        pm = Pm[idx % 4]
                        pmk = ('Pm', idx % 4)
                        tb = tmpb[idx % 2]
                        tbk = ('tmpb', idx % 2)
                        P.op('pe', lambda e: e.matmul(psum[sbk][:, 0:nq], lhsT=kt[:, ksl], rhs=qt[:, qsl], start=True, stop=True),
                             reads=QTall + KTall, writes=[PS(sbk)])
                        P.op('dve', lambda e: e.scalar_tensor_tensor(
                            out=tb[:, 0:nq], in0=psum[sbk][:, 0:nq], scalar=scale, in1=bt[:, 0:nq], op0=ALU.mult, op1=ALU.add),
                            reads=[('bt', gk)], writes=[PS(sbk), tbk])
                        P.op('act', lambda e: e.activation(out=pm[:, 0:nq], in_=tb[:, 0:nq], func=AF.Exp), reads=[tbk], writes=[pmk])

                    def stageB(idx, vs=vs, gk=gk, dil=dil, nb=nb, items=items, first_g=first_g):
                        r, jb = items[idx]
                        ti = r * nb + jb
                        t0 = r + dil * jb * 128
                        pm = Pm[idx % 4]
                        pmk = ('Pm', idx % 4)
                        nbk = 5 + idx % 3
                        rd = [('V', gk, ti), pmk, 'cst']
                        has_prev = jb > 0
                        if has_prev:
                            ppm = Pm[(idx - 1) % 4]
                            ppmk = ('Pm', (idx - 1) % 4)
                            pti = ti - 1
                            rd += [ppmk, ('V', gk, pti)]
                            P.op('pe', lambda e: e.matmul(psum[nbk][:, 0:128], lhsT=vs[:, pti, :], rhs=ppm[:, 128:256], start=True, stop=False),
                                 reads=rd, writes=[PS(nbk)])
                            P.op('pe', lambda e: e.matmul(psum[nbk][:, 128:256], lhsT=ones, rhs=ppm[:, 128:256], start=False, stop=False),
                                 reads=rd, writes=[PS(nbk)])
                        P.op('pe', lambda e: e.matmul(psum[nbk][:, 0:128], lhsT=vs[:, ti, :], rhs=pm[:, 0:128], start=(not has_prev), stop=False),
                             reads=rd, writes=[PS(nbk)])
                        P.op('pe', lambda e: e.matmul(psum[nbk][:, 128:256], lhsT=ones, rhs=pm[:, 0:128], start=False, stop=True),
                             reads=rd, writes=[PS(nbk)])
                        asl = slice(t0, t0 + dil * 127 + 1, dil)
                        src = psum[nbk][:, 0:256].rearrange("p (a n) -> p a n", a=2)
                        if first_g:
                            P.op('dve', lambda e: e.tensor_copy(out=acc[:, :, asl], in_=src), writes=[PS(nbk), 'acc'])
                        else:
                            P.op('dve', lambda e: e.tensor_tensor(out=acc[:, :, asl], in0=src, in1=acc[:, :, asl], op=ALU.add),
                                 writes=[PS(nbk), 'acc'])

                    LOOK = 2
                    n_it = len(items)
                    for i in range(min(LOOK, n_it)):
                        stageA(i)
                    for i in range(n_it):
                        if i + LOOK < n_it:
                            stageA(i + LOOK)
                        stageB(i)
                P.op('act', lambda e: e.activation(out=acc[:, 1, :], in_=acc[:, 1, :], func=AF.Ln), writes=['acc'])
                P.op('act', lambda e: e.activation(out=acc[:, 1, :], in_=acc[:, 1, :], func=AF.Exp, scale=-1.0), writes=['acc'])
                P.op('dve', lambda e: e.tensor_tensor(out=yo, in0=acc[:, 0, :], in1=acc[:, 1, :], op=ALU.mult), reads=['acc'], writes=['yo'])
                r0 = 2048 + hs * 128
                P.dma('sp', lambda e, r0=r0: e.dma_start(out=yb_d.ap()[r0:r0 + 128, :], in_=yo), reads=['yo'],
                      writes=[('ybuf', r0 // 128, Q) for Q in range(4)])

        def merge_phase(l, xsrc, xdst):
            A.reset()
            yts = [A.bf(20 * 512).rearrange("p (c n) -> p c n", c=20) for _ in range(2)]
            sg = [A.f32(512) for _ in range(2)]
            tmp = [A.f32(512) for _ in range(2)]
            macc = A.f32(8 * 512).rearrange("p (c n) -> p c n", c=8)
            mbf = A.bf(8 * 512).rearrange("p (c n) -> p c n", c=8)
            xt = [A.f32(D) for _ in range(4)]
            wl = w_in.ap()[l]
            branches = [(w_bret.ap()[l], 0, 8), (w_bdiff.ap()[l], 8, 8), (w_bdil.ap()[l], 16, 4)]

            def load_y(T):
                yt = yts[T % 2]
                tsl = slice(T * 512, (T + 1) * 512)
                src = yb_d.ap()[:, tsl].rearrange("(c p) n -> p c n", p=128)
                P.dma('sp', lambda e: e.dma_start(out=yt[:, 0:10, :], in_=src[:, 0:10, :]),
                      reads=[('ybuf', c, T) for c in range(10)], writes=[('yt', T % 2, 0)])
                P.dma('sp', lambda e: e.dma_start(out=yt[:, 10:20, :], in_=src[:, 10:20, :]),
                      reads=[('ybuf', c, T) for c in range(10, 20)], writes=[('yt', T % 2, 1)])

            load_y(0)
            for T in range(4):
                tsl = slice(T * 512, (T + 1) * 512)
                yt = yts[T % 2]
                ytk = [('yt', T % 2, 0), ('yt', T % 2, 1)]
                if T + 1 < 4:
                    load_y(T + 1)
                for ts in range(4):
                    t = T * 4 + ts
                    P.dma('sp', lambda e, ts=ts, t=t: e.dma_start(out=xt[ts], in_=xsrc[t * 128:(t + 1) * 128, :]), writes=[('xt', ts)])
                cnt = 0
                for bi, (wb, c0, nch) in enumerate(branches):
                    for hf in range(2):
                        wB, kB, oB = wload([wb[:, hf * 512:(hf + 1) * 512]])
                        gcol = 10752 + bi * 1024 + hf * 512
                        wG, kG, oG = wload([wl[:, gcol:gcol + 512]])
                        for d4 in range(4):
                            d = hf * 4 + d4
                            zb = cnt % 2
                            gb = 2 + cnt % 2
                            s_ = sg[cnt % 2]
                            sk = ('sg', cnt % 2)
                            tm = tmp[cnt % 2]
                            tmk = ('tmp', cnt % 2)
                            cnt += 1
                            for c in range(nch):
                                P.op('pe', lambda e, c=c, zb=zb, wB=wB, d4=d4, c0=c0, nch=nch, yt=yt: e.matmul(
                                    psum[zb][:, :], lhsT=wB[:, c, d4 * 128:(d4 + 1) * 128], rhs=yt[:, c0 + c, :],
                                    start=(c == 0), stop=(c == nch - 1)), reads=[kB[0]] + ytk, writes=[PS(zb)])
                            for c in range(8):
                                P.op('pe', lambda e, c=c, gb=gb, wG=wG, d4=d4, tsl=tsl: e.matmul(
                                    psum[gb][:, :], lhsT=wG[:, c, d4 * 128:(d4 + 1) * 128], rhs=hT[:, c, tsl],
                                    start=(c == 0), stop=(c == 7)), reads=[kG[0], ('hT', T)], writes=[PS(gb)])
                            P.op('act', lambda e, gb=gb, s_=s_: e.activation(out=s_, in_=psum[gb][:, :], func=AF.Sigmoid), writes=[PS(gb), sk])
                            if bi == 0:
                                P.op('dve', lambda e, zb=zb, s_=s_, d=d: e.tensor_tensor(out=macc[:, d, :], in0=psum[zb][:, :], in1=s_, op=ALU.mult),
                                     reads=[sk], writes=[PS(zb), ('macc', d)])
                            else:
                                P.op('dve', lambda e, zb=zb, s_=s_, tm=tm: e.tensor_tensor(out=tm, in0=psum[zb][:, :], in1=s_, op=ALU.mult),
                                     reads=[sk], writes=[PS(zb), tmk])
                                if bi == 1:
                                    P.op('dve', lambda e, tm=tm, d=d: e.tensor_tensor(out=macc[:, d, :], in0=macc[:, d, :], in1=tm, op=ALU.add),
                                         reads=[tmk], writes=[('macc', d)])
                                else:
                                    P.op('dve', lambda e, tm=tm, d=d: e.tensor_tensor(out=mbf[:, d, :], in0=macc[:, d, :], in1=tm, op=ALU.add),
                                         reads=[tmk, ('macc', d)], writes=[('mbf', d)])
                for ch in range(2):
                    wO, kO, oO = wload([w_out.ap()[l][:, ch * 512:(ch + 1) * 512]])
                    for ts in range(4):
                        bank = 4 + ts
                        for d in range(8):
                            P.op('pe', lambda e, d=d, ts=ts, bank=bank, wO=wO: e.matmul(
                                psum[bank][:, :], lhsT=mbf[:, d, ts * 128:(ts + 1) * 128], rhs=wO[:, d, 0:512],
                                start=(d == 0), stop=(d == 7)), reads=[kO[0], ('mbf', d)], writes=[PS(bank)])
                        P.op('dve', lambda e, ts=ts, ch=ch, bank=bank: e.tensor_tensor(
                            out=xt[ts][:, ch * 512:(ch + 1) * 512], in0=psum[bank][:, :], in1=xt[ts][:, ch * 512:(ch + 1) * 512], op=ALU.add),
                            writes=[PS(bank), ('xt', ts)])
                for ts in range(4):
                    t = T * 4 + ts
                    P.dma('sp', lambda e, ts=ts, t=t: e.dma_start(out=xdst[t * 128:(t + 1) * 128, :], in_=xt[ts]), reads=[('xt', ts)],
                          writes=[('xs', t)])

        def ffn_phase(l, xsd, nxt, odst):
            A.reset()
            xts = [[A.f32(D) for _ in range(4)] for _ in range(2)]
            sq = A.f32(D)
            hn = [A.bf(D) for _ in range(2)]
            gbc = A.f32(D)
            h2Ts = [A.bf(8 * 512).rearrange("p (c n) -> p c n", c=8) for _ in range(2)]
            aT = A.bf(22 * 512).rearrange("p (c n) -> p c n", c=22)
            sg = [A.f32(512) for _ in range(2)]
            P.dma('sp', lambda e: e.dma_start(out=gbc, in_=bcast_row(norm_ffn, l * D, D)), writes=['gbc'])
            gbc2 = A.f32(D)
            if nxt[0] == 'hT':
                hne = [A.bf(D) for _ in range(4)]
                P.dma('sp', lambda e: e.dma_start(out=gbc2, in_=bcast_row(norm_mix, nxt[1] * D, D)), writes=['gbc2'])
            else:
                ote = [A.f32(D) for _ in range(2)]
                P.dma('sp', lambda e: e.dma_start(out=gbc2, in_=bcast_row(norm_final, 0, D)), writes=['gbc2'])

            def epi_elem(T, xt):
                for ts in range(4):
                    t = T * 4 + ts
                    xk = ('xt', T % 2, ts)
                    rs, k = rms_stats(xt[ts], xk, sq, 16 + ts, RMS_EPS)
                    if nxt[0] == 'hT':
                        P.op('dve', lambda e, ts=ts, rs=rs: e.scalar_tensor_tensor(out=hne[ts], in0=xt[ts], scalar=rs, in1=gbc2,
                                                                                     op0=ALU.mult, op1=ALU.mult),
                             reads=[xk, k, 'gbc2'], writes=[('hne', ts)])
                    else:
                        ob = ote[t % 2]
                        P.op('dve', lambda e, ts=ts, rs=rs, ob=ob: e.scalar_tensor_tensor(out=ob, in0=xt[ts], scalar=rs, in1=gbc2,
                                                                                        op0=ALU.mult, op1=ALU.mult),
                             reads=[xk, k, 'gbc2'], writes=[('ote', t % 2)])
                        P.dma('sp', lambda e, ob=ob, t=t: e.dma_start(out=odst[t * 128:(t + 1) * 128, :], in_=ob), reads=[('ote', t % 2)],
                              is_output=True)

            def epi_tr(T):
                if nxt[0] != 'hT':
                    return
                pv = psum_bf[7][:, 0:1024].rearrange("p (c n) -> p c n", c=8)
                for ts in range(4):
                    t = T * 4 + ts
                    for c in range(8):
                        P.op('pe', lambda e, c=c, ts=ts: e.transpose(out=pv[:, c, :], in_=hne[ts][:, c * 128:(c + 1) * 128], identity=ident),
                             reads=[('hne', ts), 'cst'], writes=[PS(7)])
                    P.op('act', lambda e, t=t: e.activation(out=hT[:, :, t * 128:(t + 1) * 128], in_=pv, func=AF.Copy),
                         writes=[PS(7), ('hT', t // 4)])
            wg = w_g.ap()[l]
            wu = w_u.ap()[l]
            wd = w_d.ap()[l]

            def load_x(T):
                for ts in range(4):
                    t = T * 4 + ts
                    P.dma('sp', lambda e, ts=ts, t=t: e.dma_start(out=xts[T % 2][ts], in_=xsd[t * 128:(t + 1) * 128, :]),
                          reads=[('xs', t)], writes=[('xt', T % 2, ts)])

            def norm_elem(T, ts):
                t = T * 4 + ts
                xk = ('xt', T % 2, ts)
                xb = xts[T % 2][ts]
                rs, k = rms_stats(xb, xk, sq, 8 + (t % 8), RMS_EPS)
                hb = hn[t % 2]
                hk = ('hn', t % 2)
                P.op('dve', lambda e: e.scalar_tensor_tensor(out=hb, in0=xb, scalar=rs, in1=gbc, op0=ALU.mult, op1=ALU.mult),
                     reads=[xk, k, 'gbc'], writes=[hk])

            def norm_tr(T, ts):
                t = T * 4 + ts
                hb = hn[t % 2]
                hk = ('hn', t % 2)
                h2T = h2Ts[T % 2]
                pv = psum_bf[7][:, 0:1024].rearrange("p (c n) -> p c n", c=8)
                for c in range(8):
                    P.op('pe', lambda e, c=c: e.transpose(out=pv[:, c, :], in_=hb[:, c * 128:(c + 1) * 128], identity=ident),
                         reads=[hk, 'cst'], writes=[PS(7)])
                P.op('act', lambda e: e.activation(out=h2T[:, :, ts * 128:(ts + 1) * 128], in_=pv, func=AF.Copy),
                     writes=[PS(7), ('h2T', T % 2)])

            load_x(0)
            for ts in range(4):
                norm_elem(0, ts)
                norm_tr(0, ts)
            for T in range(4):
                xt = xts[T % 2]
                h2T = h2Ts[T % 2]
                h2k = ('h2T', T % 2)
                if T + 1 < 4:
                    load_x(T + 1)
                cnt = 0
                for gi_, f0 in enumerate(range(0, FFN, 512)):
                    nf = min(512, FFN - f0)
                    wG, kG, oG = wload([wg[:, f0:f0 + nf]])
                    wU, kU, oU = wload([wu[:, f0:f0 + nf]])
                    for f4 in range(nf // 128):
                        fc = f0 // 128 + f4
                        gb = cnt % 2
                        ub = 2 + cnt % 2
                        s_ = sg[cnt % 2]
                        sk = ('sg', cnt % 2)
                        cnt += 1
                        for c in range(8):
                            P.op('pe', lambda e, c=c, gb=gb, wG=wG, f4=f4, h2T=h2T: e.matmul(
                                psum[gb][:, :], lhsT=wG[:, c, f4 * 128:(f4 + 1) * 128], rhs=h2T[:, c, :], start=(c == 0), stop=(c == 7)),
                                reads=[kG[0], h2k], writes=[PS(gb)])
                        for c in range(8):
                            P.op('pe', lambda e, c=c, ub=ub, wU=wU, f4=f4, h2T=h2T: e.matmul(
                                psum[ub][:, :], lhsT=wU[:, c, f4 * 128:(f4 + 1) * 128], rhs=h2T[:, c, :], start=(c == 0), stop=(c == 7)),
                                reads=[kU[0], h2k], writes=[PS(ub)])
                        P.op('act', lambda e, gb=gb, s_=s_: e.activation(out=s_, in_=psum[gb][:, :], func=AF.Silu), writes=[PS(gb), sk])
                        P.op('dve', lambda e, ub=ub, s_=s_, fc=fc: e.tensor_tensor(out=aT[:, fc, :], in0=psum[ub][:, :], in1=s_, op=ALU.mult),
                             reads=[sk], writes=[PS(ub), ('aT', fc)])
                    if gi_ == 0 and T > 0:
                        epi_tr(T - 1)
                    if T + 1 < 4:
                        if gi_ < 4:
                            norm_elem(T + 1, gi_)
                        if 1 <= gi_ <= 4:
                            norm_tr(T + 1, gi_ - 1)
                for ch in range(2):
                    for g3, (fa, fb) in enumerate(((0, 8), (8, 16), (16, 22))):
                        nch = fb - fa
                        wD, kD, oD = wload([wd[fa * 128:fb * 128, ch * 512:(ch + 1) * 512]])
                        for ts in range(4):
                            bank = 4 + ts
                            for c in range(nch):
                                fc = fa + c
                                P.op('pe', lambda e, c=c, fc=fc, ts=ts, bank=bank, wD=wD: e.matmul(
                                    psum[bank][:, :], lhsT=aT[:, fc, ts * 128:(ts + 1) * 128], rhs=wD[:, c, 0:512],
                                    start=(fc == 0), stop=(fc == 21)), reads=[kD[0], ('aT', fc)], writes=[PS(bank)])
                    for ts in range(4):
                        bank = 4 + ts
                        P.op('dve', lambda e, ts=ts, ch=ch, bank=bank, xt=xt: e.tensor_tensor(
                            out=xt[ts][:, ch * 512:(ch + 1) * 512], in0=psum[bank][:, :], in1=xt[ts][:, ch * 512:(ch + 1) * 512], op=ALU.add),
                            writes=[PS(bank), ('xt', T % 2, ts)])
                if nxt[0] == 'hT':
                    for ts in range(4):
                        t = T * 4 + ts
                        P.dma('sp', lambda e, ts=ts, t=t, xt=xt: e.dma_start(out=xsd[t * 128:(t + 1) * 128, :], in_=xt[ts]),
                              reads=[('xt', T % 2, ts)], writes=[('xs', t)])
                epi_elem(T, xt)
            epi_tr(3)

        def final_phase(xsd, odst):
            A.reset()
            xt = [A.f32(D) for _ in range(3)]
            sq = A.f32(D)
            ot = [A.f32(D) for _ in range(2)]
            gbc = A.f32(D)
            P.dma('sp', lambda e: e.dma_start(out=gbc, in_=bcast_row(norm_final, 0, D)), writes=['gbc'])
            for t in range(16):
                xb = xt[t % 3]
                xk = ('xt', t % 3)
                P.dma('sp', lambda e, xb=xb, t=t: e.dma_start(out=xb, in_=xsd[t * 128:(t + 1) * 128, :]), reads=[('xs', t)], writes=[xk])
                rs, k = rms_stats(xb, xk, sq, t % 8, RMS_EPS)
                ob = ot[t % 2]
                ok_ = ('ot', t % 2)
                P.op('dve', lambda e, ob=ob, xb=xb, rs=rs: e.scalar_tensor_tensor(
                    out=ob, in0=xb, scalar=rs, in1=gbc, op0=ALU.mult, op1=ALU.mult), reads=[xk, k, 'gbc'], writes=[ok_])
                P.dma('sp', lambda e, ob=ob, t=t: e.dma_start(out=odst[t * 128:(t + 1) * 128, :], in_=ob), reads=[ok_], is_output=True)

        for s in range(n_seq):
            for li, l in enumerate(layers):
                xin = x_d.ap()[s] if li == 0 else xs_d.ap()[s]
                xsd = xs_d.ap()[s]
                if "n1" in phases and (li == 0 or "ffn" not in phases):
                    norm_phase_hT(xin, norm_mix, l * D)
                    P.fence()
                if "ret" in phases:
                    retention_phase(l)
                    P.fence()
                if "diff" in phases:
                    diff_phase(l)
                    P.fence()
                if "dil" in phases:
                    dil_phase(l)
                    P.fence()
                if "merge" in phases:
                    merge_phase(l, xin, xsd)
                    P.fence()
                if "ffn" in phases:
                    last = (li == len(layers) - 1)
                    ffn_phase(l, xsd, ('final',) if last else ('hT', layers[li + 1]), out_d.ap()[s])
                    P.fence()
        P.finish()
        P.emit()
    return nc, P


WNAMES = ["w_in", "w_branch_ret", "w_branch_diff", "w_branch_dil", "w_out", "norm_mix", "norm_ffn", "ret_gn_gain",
          "diff_lambda", "diff_subln_gain", "w_ffn_gate", "w_ffn_up", "w_ffn_down", "norm_final"]


def make_in_maps(inputs, n_cores=NCORES, n_seq=SEQ_PER_CORE):
    consts = _host_consts(inputs["rel_bias"])
    shared = {k: np.ascontiguousarray(np.asarray(inputs[k], dtype=np.float32)) for k in WNAMES}
    shared.update(consts)
    x = np.asarray(inputs["x"], dtype=np.float32)
    maps = []
    for i in range(n_cores):
        m = dict(shared)
        m["x"] = np.ascontiguousarray(x[i * n_seq:(i + 1) * n_seq])
        maps.append(m)
    return maps


def kernel(**inputs):
    nc, P = build_program()
    maps = make_in_maps(inputs)
    res = run_bass_kernel_spmd(nc, maps, core_ids=list(range(NCORES)))
    out = np.concatenate([np.asarray(r["out"]) for r in res.results], axis=0)
    return out.astype(np.float32)
```

```python
import math
import contextlib
import numpy as np
import ml_dtypes
import concourse.bass as bass
import concourse.mybir as mybir
from concourse.bass_utils import run_bass_kernel_spmd

F32 = mybir.dt.float32
BF16 = mybir.dt.bfloat16
AF = mybir.ActivationFunctionType
ALU = mybir.AluOpType
ENGS = ['pe', 'act', 'dve', 'pool', 'sp']

D = 1024
S = 2048
NCORES = 8
SEQ_PER_CORE = 2
DEPTH = 2
IN_WIDTH = 13824
FFN = 2816
MASKV = -30000.0
RMS_EPS = 1e-6
GN_EPS = 1e-5
DIFF_EPS = 1e-5
DIL = ((128, 1), (512, 4), (2048, 16))
import os as _os
DILG = [int(v) for v in _os.environ.get('DILG', '0,1,2').split(',')]


class Prog:
    def __init__(self, nc, n_dma_sems=24, same_eng_sync=True):
        self.nc = nc
        self.ops = {e: [] for e in ENGS}
        self.res = {}
        self.ndma = 0
        self.ndmaq = {}
        self.ND = n_dma_sems
        self.same = same_eng_sync
        self.out_tokens = []
        self.last_compute = {}
        self.dma_since_fence = []

    def _deps(self, reads, writes):
        deps = set()
        for r in reads:
            st = self.res.get(r)
            if st and st['w'] is not None:
                deps.add(st['w'])
        for w in writes:
            st = self.res.get(w)
            if st:
                if st['w'] is not None:
                    deps.add(st['w'])
                deps.update(st['r'])
        return deps

    def _commit(self, tok, reads, writes):
        for r in reads:
            st = self.res.setdefault(r, {'w': None, 'r': set()})
            st['r'] = {t for t in st['r'] if not (t[0] == tok[0] and t[0] != 'dma')} | {tok}
        for w in writes:
            self.res[w] = {'w': tok, 'r': set()}

    def op(self, eng, fn, reads=(), writes=()):
        deps = self._deps(reads, writes)
        idx = len(self.ops[eng])
        tok = (eng, idx)
        self.ops[eng].append(dict(fn=fn, deps=deps, dma=None))
        self._commit(tok, reads, writes)
        self.last_compute[eng] = tok
        return tok

    def dma(self, queue, fn, reads=(), writes=(), is_output=False):
        deps = self._deps(reads, writes)
        k = self.ndmaq.get(queue, 0)
        self.ndmaq[queue] = k + 1
        self.ndma += 1
        if k >= self.ND:
            deps.add(('dma', (queue, k - self.ND)))
        self.ops[queue].append(dict(fn=fn, deps=deps, dma=(queue, k)))
        tok = ('dma', (queue, k))
        self._commit(tok, reads, writes)
        self.dma_since_fence.append(tok)
        if is_output:
            self.out_tokens.append(tok)
        return tok

    def fence(self):
        deps = set(self.last_compute.values())
        for q in ('sp', 'pool'):
            deps |= set([t for t in self.dma_since_fence if t[1][0] == q][-self.ND:])
        self.dma_since_fence = []
        for e in ('act', 'dve', 'sp'):
            self.ops[e].append(dict(fn=None, deps=set(deps), dma=None))

    def finish(self):
        self.ops['sp'].append(dict(fn=None, deps=set(self.out_tokens[-self.ND:]), dma=None))

    def emit(self):
        nc = self.nc
        same = self.same
        ND = self.ND
        need = {e: set() for e in ENGS}
        for e in ENGS:
            for o in self.ops[e]:
                for d in o['deps']:
                    if d[0] == 'dma':
                        continue
                    if d[0] == e and (e == 'pe' or not same):
                        continue
                    need[d[0]].add(d[1])
        tick = {e: {} for e in ENGS}
        for e in ENGS:
            c = 0
            for i in range(len(self.ops[e])):
                if i in need[e]:
                    c += 1
                    tick[e][i] = c
        self.stats = {e: [len(self.ops[e]), len(need[e])] for e in ENGS}
        with contextlib.ExitStack() as st:
            esem = {e: st.enter_context(nc.semaphore("s_" + e)) for e in ENGS}
            dsem = {q: [st.enter_context(nc.semaphore("d_%s_%d" % (q, i))) for i in range(ND)] for q in ('sp', 'pool')}
            block = st.enter_context(nc.Block())

            def mk(e):
                def body(eng):
                    waited = {}
                    nw = 0
                    for i, o in enumerate(self.ops[e]):
                        for d in sorted(o['deps']):
                            if d[0] == 'dma':
                                dq, dk = d[1]
                                key = ('dma', dq, dk % ND)
                                v = 16 * (dk // ND + 1)
                                sem = dsem[dq][dk % ND]
                            else:
                                if d[0] == e and (e == 'pe' or not same):
                                    continue
                                key = d[0]
                                v = tick[d[0]][d[1]]
                                sem = esem[d[0]]
                            if waited.get(key, 0) >= v:
                                continue
                            waited[key] = v
                            eng.wait_ge(sem, v)
                            nw += 1
                        if o['fn'] is None:
                            continue
                        ins = o['fn'](eng)
                        if o['dma'] is not None:
                            ins.then_inc(dsem[o['dma'][0]][o['dma'][1] % ND], 16)
                        elif i in need[e]:
                            ins.then_inc(esem[e], 1)
                    self.stats[e].append(nw)
                return body

            block.tensor(mk('pe'))
            block.scalar(mk('act'))
            block.vector(mk('dve'))
            block.gpsimd(mk('pool'))
            block.sync(mk('sp'))


def _rel_bucket(n):
    n = np.maximum(n, 0)
    exact = 16
    lr = np.log(np.maximum(n, exact).astype(np.float32) / np.float32(exact)) / np.float32(math.log(128 / 16))
    large = np.minimum(exact + (lr * np.float32(16)).astype(np.int32), 31)
    return np.where(n < exact, n, large)


def _host_consts(rel_bias):
    c = {}
    ident = np.eye(128, dtype=np.float32)
    ones = np.ones((128, 128), np.float32)
    perm = np.zeros((128, 128), np.float32)
    for m in range(128):
        perm[(m + 64) % 128, m] = 1.0
    c["cst_bf"] = np.stack([ident, ones, perm], axis=1).astype(ml_dtypes.bfloat16)
    half = 64
    inv = (np.float32(10000.0) ** (-np.arange(half, dtype=np.float32) / np.float32(half))).astype(np.float32)
    pos = np.arange(S, dtype=np.float32)
    ang = (pos[:, None] * inv[None, :]).astype(np.float32)
    cos = np.cos(ang).astype(np.float32).T
    sin = np.sin(ang).astype(np.float32).T
    c["rot"] = np.stack([np.concatenate([cos, cos], 0), np.concatenate([-sin, sin], 0)], 0).astype(np.float32)
    rc = np.zeros((4, 128, 644), np.float32)
    i = np.arange(128, dtype=np.float64)
    for h in range(4):
        lg = math.log1p(-2.0 ** (-5.0 - h))
        rel = i[None, :] - i[:, None]
        rc[h, :, 0:128] = np.where(rel >= 0, np.exp(lg * np.maximum(rel, 0.0)), 0.0)
        rc[h, :, 128:640] = np.tile(np.exp((i + 1.0) * lg)[None, :], (128, 4))
        rc[h, :, 640] = np.exp((127.0 - i) * lg)
        rc[h, :, 641] = math.exp(128.0 * lg)
    c["rconst"] = rc
    rb = np.asarray(rel_bias, np.float32)
    kj = np.arange(128)[:, None]
    qi = np.arange(256)[None, :]
    dist = qi - kj
    db = np.zeros((8, 128, 256), np.float32)
    bk = _rel_bucket(dist)
    for h in range(8):
        db[h] = np.where(dist >= 0, rb[bk, h], np.float32(MASKV))
    c["dbias"] = db
    c["cbias"] = np.ascontiguousarray(np.broadcast_to(rb[31:32, :], (128, 20))).astype(np.float32)
    lb = np.zeros((12, 128, 256), np.float32)
    for g, (win, dil) in enumerate(DIL):
        m = qi - kj
        valid = (m >= 0) & (m <= win // dil)
        bk = _rel_bucket(m * dil)
        for hs in range(4):
            lb[g * 4 + hs] = np.where(valid, rb[bk, 8 + g * 4 + hs], np.float32(MASKV))
    c["lbias"] = lb
    return c


def build_program(n_seq=SEQ_PER_CORE, layers=(0, 1), phases=("n1", "ret", "diff", "dil", "merge", "ffn", "final"),
                  dbg=False, ret_heads=range(4), diff_heads=range(8), dil_slots=range(4), same_eng_sync=True):
    nc = bass.Bass("TRN2", target_bir_lowering=False)
    P = Prog(nc, same_eng_sync=same_eng_sync)

    def din(name, shape, dt=F32):
        return nc.dram_tensor(name, list(shape), dt, kind="ExternalInput")

    x_d = din("x", [n_seq, S, D])
    w_in = din("w_in", [DEPTH, D, IN_WIDTH])
    w_bret = din("w_branch_ret", [DEPTH, 1024, D])
    w_bdiff = din("w_branch_diff", [DEPTH, 1024, D])
    w_bdil = din("w_branch_dil", [DEPTH, 512, D])
    w_out = din("w_out", [DEPTH, D, D])
    norm_mix = din("norm_mix", [DEPTH, D])
    norm_ffn = din("norm_ffn", [DEPTH, D])
    gn_gain = din("ret_gn_gain", [DEPTH, 1024])
    dlam = din("diff_lambda", [DEPTH, 4, 64])
    subln = din("diff_subln_gain", [DEPTH, 128])
    w_g = din("w_ffn_gate", [DEPTH, D, FFN])
    w_u = din("w_ffn_up", [DEPTH, D, FFN])
    w_d = din("w_ffn_down", [DEPTH, FFN, D])
    norm_final = din("norm_final", [D])
    cst_bf_d = din("cst_bf", [128, 3, 128], BF16)
    rot_d = din("rot", [2, 128, S])
    rconst_d = din("rconst", [4, 128, 644])
    dbias_d = din("dbias", [8, 128, 256])
    cbias_d = din("cbias", [128, 20])
    lbias_d = din("lbias", [12, 128, 256])
    okind = "ExternalOutput" if dbg else "Internal"
    out_d = nc.dram_tensor("out", [n_seq, S, D], F32, kind="ExternalOutput")
    xs_d = nc.dram_tensor("xs", [n_seq, S, D], F32, kind=okind)
    yb_d = nc.dram_tensor("ybuf", [2560, S], BF16, kind=okind)

    st = contextlib.ExitStack()
    with st:
        def sb(name, shape, dt):
            return st.enter_context(nc.sbuf_tensor(name, shape, dt))

        cst = sb("cst", [128, 3, 128], BF16)
        ident = cst[:, 0, :]
        ones = cst[:, 1, :]
        perm = cst[:, 2, :]
        hT = sb("hT", [128, 8, S], BF16)
        NW = 4
        wring = [sb("wr%d" % i, [128, 8, 512], BF16) for i in range(NW)]
        cbias = sb("cbias_sb", [128, 20], F32)
        smalls = sb("smalls", [128, 64], F32)
        ARENA = 100 * 1024
        arena_bf = sb("arena", [128, ARENA // 2], BF16)
        arena_f = arena_bf.bitcast(F32)
        psum = [st.enter_context(nc.psum_tensor("ps%d" % i, [128, 512], F32)) for i in range(8)]
        psum_bf = [p.bitcast(BF16) for p in psum]

        class Arena:
            def __init__(self):
                self.off = 0

            def reset(self, off=0):
                self.off = off

            def f32(self, n):
                o = self.off
                self.off += ((n * 4 + 63) // 64) * 64
                assert self.off <= ARENA, ("arena overflow", self.off)
                return arena_f[:, o // 4:o // 4 + n]

            def bf(self, n):
                o = self.off
                self.off += ((n * 2 + 63) // 64) * 64
                assert self.off <= ARENA, ("arena overflow", self.off)
                return arena_bf[:, o // 2:o // 2 + n]

        A = Arena()
        wstate = {'n': 0}

        def PS(b):
            return ('ps', b)

        def wload(pieces):
            slot = wstate['n'] % NW
            wstate['n'] += 1
            key = ('w', slot)
            offs = []
            c0 = 0
            for src in pieces:
                rows, ncols = src.shape
                nch = rows // 128
                v = src.rearrange("(c p) n -> p c n", p=128)
                P.dma('pool', lambda e, v=v, slot=slot, nch=nch, c0=c0, ncols=ncols:
                      e.dma_start(out=wring[slot][:, 0:nch, c0:c0 + ncols], in_=v),
                      writes=([(key, i) for i in range(4)] if len(offs) == 0 else [(key, len(offs))]))
                offs.append(c0)
                c0 += ncols
            assert c0 <= 512
            keys = [(key, i) for i in range(len(offs))]
            return wring[slot], keys, offs

        epsg = smalls[:, 60:61]
        epsd = smalls[:, 61:62]
        P.op('dve', lambda e: e.memset(smalls[:, 60:61], GN_EPS), writes=['epsv'])
        P.op('dve', lambda e: e.memset(smalls[:, 61:62], DIFF_EPS), writes=['epsv'])
        P.dma('sp', lambda e: e.dma_start(out=cst[:, :, :], in_=cst_bf_d.ap()), writes=['cst'])
        P.dma('sp', lambda e: e.dma_start(out=cbias[:, :], in_=cbias_d.ap()), writes=['cbias'])

        def bcast_row(t, offset, n):
            return bass.AP(t, offset, [[0, 128], [1, n]])

        def col_vec(t, offset, n=128):
            return bass.AP(t, offset, [[1, n], [1, 1]])

        def rms_stats(xtile, xkey, sq, idx, eps):
            ss = smalls[:, idx * 3:idx * 3 + 1]
            sd = smalls[:, idx * 3 + 1:idx * 3 + 2]
            rs = smalls[:, idx * 3 + 2:idx * 3 + 3]
            k = ('st', idx)
            P.op('act', lambda e: e.activation(out=sq, in_=xtile, func=AF.Square, accum_out=ss),
                 reads=[xkey], writes=['sq', k])
            P.op('act', lambda e: e.activation(out=sd, in_=ss, func=AF.Sqrt, scale=1.0 / D, bias=eps),
                 reads=[], writes=[k])
            P.op('dve', lambda e: e.reciprocal(out=rs, in_=sd), reads=[], writes=[k])
            return rs, k

        def norm_phase_hT(xsrc, gain_t, gain_off):
            A.reset()
            xt = [A.f32(D) for _ in range(3)]
            sq = A.f32(D)
            hn = [A.bf(D) for _ in range(2)]
            gbc = A.f32(D)
            P.dma('sp', lambda e: e.dma_start(out=gbc, in_=bcast_row(gain_t, gain_off, D)), writes=['gbc'])
            for t in range(16):
                xb = xt[t % 3]
                xk = ('xt', t % 3)
                P.dma('sp', lambda e, xb=xb, t=t: e.dma_start(out=xb, in_=xsrc[t * 128:(t + 1) * 128, :]), writes=[xk])
                rs, k = rms_stats(xb, xk, sq, t % 8, RMS_EPS)
                hb = hn[t % 2]
                hk = ('hn', t % 2)
                P.op('dve', lambda e, hb=hb, xb=xb, rs=rs: e.scalar_tensor_tensor(
                    out=hb, in0=xb, scalar=rs, in1=gbc, op0=ALU.mult, op1=ALU.mult),
                    reads=[xk, k, 'gbc'], writes=[hk])
                b = t % 2
                pv = psum_bf[b][:, 0:1024].rearrange("p (c n) -> p c n", c=8)
                for c in range(8):
                    P.op('pe', lambda e, pv=pv, hb=hb, c=c: e.transpose(out=pv[:, c, :], in_=hb[:, c * 128:(c + 1) * 128],
                                                                       identity=ident), reads=[hk, 'cst'], writes=[PS(b)])
                P.op('act', lambda e, pv=pv, t=t: e.activation(out=hT[:, :, t * 128:(t + 1) * 128], in_=pv, func=AF.Copy),
                     writes=[PS(b), ('hT', t // 4)])

        HT4 = [('hT', j) for j in range(4)]

        def proj_fm(wt, wkeys_i, col0, ncols, j, bank, evac):
            for c in range(8):
                P.op('pe', lambda e, c=c: e.matmul(psum[bank][0:ncols, :], lhsT=wt[:, c, col0:col0 + ncols],
                                                   rhs=hT[:, c, j * 512:(j + 1) * 512], start=(c == 0), stop=(c == 7)),
                     reads=[wkeys_i, ('hT', j)], writes=[PS(bank)])
            evac(psum[bank][0:ncols, :], PS(bank))

        def retention_phase(l):
            A.reset()
            cosT = A.f32(S)
            sinT = A.f32(S)
            rcs = [A.f32(644) for _ in range(2)]
            gnv = A.f32(8)
            sgT = A.bf(2 * S).rearrange("p (a n) -> p a n", a=2)
            Qp = A.bf(S)
            Qpp = A.bf(S)
            Kp = A.bf(S)
            raw = [A.bf(512) for _ in range(2)]
            Kpp = A.bf(16 * 128).rearrange("p (n d) -> p n d", n=16)
            V = A.bf(16 * 256).rearrange("p (n e) -> p n e", n=16)
            Sbf = A.bf(16 * 256).rearrange("p (n e) -> p n e", n=16)
            Sst2 = [A.f32(256) for _ in range(2)]
            PT = [A.bf(128) for _ in range(4)]
            t1 = [A.f32(512) for _ in range(2)]
            t2 = [A.f32(512) for _ in range(2)]
            ysq = [A.bf(2 * 512).rearrange("p (a n) -> p a n", a=2) for _ in range(2)]
            ybf = [A.bf(2 * 512).rearrange("p (a n) -> p a n", a=2) for _ in range(2)]
            mean = A.f32(512)
            var = A.f32(512)
            msq = A.f32(512)
            tt = [A.f32(512) for _ in range(2)]
            yout = [A.bf(2 * 512).rearrange("p (a n) -> p a n", a=2) for _ in range(2)]

            P.dma('sp', lambda e: e.dma_start(out=cosT, in_=rot_d.ap()[0]), writes=['cosT'])
            P.dma('sp', lambda e: e.dma_start(out=sinT, in_=rot_d.ap()[1]), writes=['sinT'])
            P.dma('sp', lambda e: e.dma_start(out=gnv.rearrange("p (c o) -> p c o", o=1),
                                              in_=bass.AP(gn_gain, l * 1024, [[1, 128], [128, 8], [1, 1]]),
                                              allow_slow_non_contiguous=True), writes=['gnv'])
            wl = w_in.ap()[l]
            for h in ret_heads:
                rc = rcs[h % 2]
                rck = ('rc', h % 2)
                P.dma('sp', lambda e, rc=rc, h=h: e.dma_start(out=rc, in_=rconst_d.ap()[h]), writes=[rck])
                decT = rc[:, 0:128]
                qdec = rc[:, 128:640]
                kdec = rc[:, 640:641]
                gam = rc[:, 641:642]
                wA, kA, oA = wload([wl[:, h * 128:(h + 1) * 128], wl[:, 512 + h * 128:512 + (h + 1) * 128],
                                    wl[:, 1024 + h * 256:1024 + (h + 1) * 256]])
                wB, kB, oB = wload([wl[:, 2048 + h * 256:2048 + (h + 1) * 256]])
                tiles = [(w_, j) for w_ in ("q", "k") for j in range(4)]

                def rot_proj(i, wA=wA, kA=kA, oA=oA):
                    which, j = tiles[i]
                    bank = i % 3
                    rb_ = raw[i % 2]
                    rk = ('raw', i % 2)
                    scale = 1.0 if which == "q" else 128.0 ** -0.5

                    def evac(ps_ap, psk):
                        P.op('act', lambda e: e.activation(out=rb_, in_=ps_ap, func=AF.Copy, scale=scale), writes=[psk, rk])
                    wi = 0 if which == "q" else 1
                    proj_fm(wA, kA[wi], oA[wi], 128, j, bank, evac)

                def rot_post(i, qdec=qdec, rck=rck):
                    which, j = tiles[i]
                    swb = 3 + i % 2
                    rb_ = raw[i % 2]
                    rk = ('raw', i % 2)
                    a1 = t1[i % 2]
                    a2 = t2[i % 2]
                    tk = ('t12', i % 2)
                    sl = slice(j * 512, (j + 1) * 512)
                    P.op('pe', lambda e: e.matmul(psum[swb][:, :], lhsT=perm, rhs=rb_, start=True, stop=True),
                         reads=[rk, 'cst'], writes=[PS(swb)])
                    P.op('dve', lambda e: e.tensor_tensor(out=a1, in0=rb_, in1=cosT[:, sl], op=ALU.mult),
                         reads=[rk, 'cosT'], writes=[tk])
                    P.op('dve', lambda e: e.tensor_tensor(out=a2, in0=psum[swb][:, :], in1=sinT[:, sl], op=ALU.mult),
                         reads=['sinT'], writes=[PS(swb), (tk, 2)])
                    P.op('dve', lambda e: e.tensor_tensor(out=a1, in0=a1, in1=a2, op=ALU.add),
                         reads=[(tk, 2)], writes=[tk])
                    if which == "q":
                        P.op('act', lambda e: e.activation(out=Qp[:, sl], in_=a1, func=AF.Copy), reads=[tk], writes=[('Qp', j)])
                        P.op('dve', lambda e: e.tensor_tensor(out=Qpp[:, sl], in0=a1, in1=qdec, op=ALU.mult),
                             reads=[tk, rck], writes=[('Qpp', j)])
                    else:
                        P.op('act', lambda e: e.activation(out=Kp[:, sl], in_=a1, func=AF.Copy), reads=[tk], writes=[('Kp', j)])

                rot_proj(0)
                for i in range(8):
                    if i + 1 < 8:
                        rot_proj(i + 1)
                    rot_post(i)
                for t in range(16):
                    bank = t % 3
                    for c in range(8):
                        P.op('pe', lambda e, c=c, t=t, bank=bank, wA=wA, oA=oA: e.matmul(
                            psum[bank][:, 0:256], lhsT=hT[:, c, t * 128:(t + 1) * 128],
                            rhs=wA[:, c, oA[2]:oA[2] + 256], start=(c == 0), stop=(c == 7)),
                            reads=[kA[2], ('hT', t // 4)], writes=[PS(bank)])
                    P.op('act', lambda e, t=t, bank=bank: e.activation(out=V[:, t, :], in_=psum[bank][:, 0:256], func=AF.Copy),
                         writes=[PS(bank), ('V', t)])
                for half in range(2):
                    bank = 5 + half
                    pv = psum_bf[bank][:, 0:1024].rearrange("p (n d) -> p n d", n=8)
                    for n8 in range(8):
                        n = half * 8 + n8
                        P.op('pe', lambda e, pv=pv, n8=n8, n=n: e.transpose(out=pv[:, n8, :], in_=Kp[:, n * 128:(n + 1) * 128], identity=ident),
                             reads=[('Kp', n // 4), 'cst'], writes=[PS(bank)])
                    P.op('dve', lambda e, pv=pv, half=half, kdec=kdec: e.tensor_scalar(
                        out=Kpp[:, half * 8:(half + 1) * 8, :], in0=pv, scalar1=kdec, scalar2=None, op0=ALU.mult),
                        reads=[rck], writes=[PS(bank), ('Kpp', half)])

                def kv_step(n, gam=gam, rck=rck):
                    bank = 3 + n % 2
                    snew = Sst2[(n + 1) % 2]
                    sold = Sst2[n % 2]
                    P.op('pe', lambda e: e.matmul(psum[bank][:, 0:256], lhsT=Kpp[:, n, :], rhs=V[:, n, :], start=True, stop=True),
                         reads=[('Kpp', n // 8), ('V', n)], writes=[PS(bank)])
                    if n == 0:
                        P.op('dve', lambda e: e.tensor_copy(out=snew, in_=psum[bank][:, 0:256]), writes=[PS(bank), ('Sst', (n + 1) % 2)])
                    else:
                        P.op('dve', lambda e: e.scalar_tensor_tensor(
                            out=snew, in0=sold, scalar=gam, in1=psum[bank][:, 0:256], op0=ALU.mult, op1=ALU.add),
                            reads=[rck, ('Sst', n % 2)], writes=[PS(bank), ('Sst', (n + 1) % 2)])
                    P.op('act', lambda e: e.activation(out=Sbf[:, n + 1, :], in_=snew, func=AF.Copy),
                         reads=[('Sst', (n + 1) % 2)], writes=[('Sbf', n + 1)])

                gi_ = 0
                for a in range(2):
                    for j in range(4):
                        bank = gi_ % 3

                        def evac(ps_ap, psk, a=a, j=j):
                            P.op('act', lambda e: e.activation(out=sgT[:, a, j * 512:(j + 1) * 512], in_=ps_ap, func=AF.Silu),
                                 writes=[psk, ('sgT', a, j)])
                        proj_fm(wB, kB[0], oB[0] + a * 128, 128, j, bank, evac)
                        for n in (2 * gi_, 2 * gi_ + 1):
                            if n < 15:
                                kv_step(n)
                        gi_ += 1
                def ybanks(T):
                    return (6, 7) if T % 2 == 0 else (2, 3)

                def stA(n, decT=decT, rck=rck):
                    sb_ = 4 + n % 2
                    pt = PT[n % 4]
                    ptk = ('PT', n % 4)
                    csl = slice(n * 128, (n + 1) * 128)
                    P.op('pe', lambda e: e.matmul(psum[sb_][:, 0:128], lhsT=Kp[:, csl], rhs=Qp[:, csl], start=True, stop=True),
                         reads=[('Kp', n // 4), ('Qp', n // 4)], writes=[PS(sb_)])
                    P.op('dve', lambda e: e.tensor_tensor(out=pt, in0=psum[sb_][:, 0:128], in1=decT, op=ALU.mult),
                         reads=[rck], writes=[PS(sb_), ptk])

                def stB(n):
                    T, nn = n // 4, n % 4
                    yb = ybanks(T)
                    pt = PT[n % 4]
                    ptk = ('PT', n % 4)
                    csl = slice(n * 128, (n + 1) * 128)
                    osl = slice(nn * 128, (nn + 1) * 128)
                    for a in range(2):
                        P.op('pe', lambda e, a=a: e.matmul(
                            psum[yb[a]][:, osl], lhsT=V[:, n, a * 128:(a + 1) * 128], rhs=pt, start=True, stop=(n == 0)),
                            reads=[('V', n), ptk], writes=[PS(yb[a])])
                        if n > 0:
                            P.op('pe', lambda e, a=a: e.matmul(
                                psum[yb[a]][:, osl], lhsT=Sbf[:, n, a * 128:(a + 1) * 128], rhs=Qpp[:, csl], start=False, stop=True),
                                reads=[('Sbf', n), ('Qpp', n // 4)], writes=[PS(yb[a])])

                def g1(T):
                    yb = ybanks(T)
                    ysq_ = ysq[T % 2]
                    ybf_ = ybf[T % 2]
                    yk = ('ysq', T % 2)
                    bk_ = ('ybf', T % 2)
                    for a in range(2):
                        P.op('act', lambda e, a=a: e.activation(out=ysq_[:, a, :], in_=psum[yb[a]][:, :], func=AF.Square),
                             writes=[PS(yb[a]), (yk, a)])
                        P.op('dve', lambda e, a=a: e.tensor_copy(out=ybf_[:, a, :], in_=psum[yb[a]][:, :]),
                             writes=[PS(yb[a]), (bk_, a)])

                def g2_steps(T, h=h):
                    ysq_ = ysq[T % 2]
                    ybf_ = ybf[T % 2]
                    yk = ('ysq', T % 2)
                    bk_ = ('ybf', T % 2)
                    tsl = slice(T * 512, (T + 1) * 512)
                    yo = yout[T % 2]
                    yok = ('yout', T % 2)

                    def s1():
                        for a in range(2):
                            P.op('pe', lambda e, a=a: e.matmul(psum[0][:, :], lhsT=ones, rhs=ybf_[:, a, :], start=(a == 0), stop=(a == 1)),
                                 reads=[(bk_, a), 'cst'], writes=[PS(0)])
                        for a in range(2):
                            P.op('pe', lambda e, a=a: e.matmul(psum[1][:, :], lhsT=ones, rhs=ysq_[:, a, :], start=(a == 0), stop=(a == 1)),
                                 reads=[(yk, a), 'cst'], writes=[PS(1)])
                        P.op('act', lambda e: e.activation(out=mean, in_=psum[0][:, :], func=AF.Copy, scale=1.0 / 256),
                             writes=[PS(0), 'mean'])
                        P.op('act', lambda e: e.activation(out=msq, in_=psum[0][:, :], func=AF.Square, scale=1.0 / 256),
                             writes=[PS(0), 'msq'])
                        P.op('dve', lambda e: e.scalar_tensor_tensor(out=var, in0=psum[1][:, :], scalar=1.0 / 256, in1=msq,
                                                                     op0=ALU.mult, op1=ALU.subtract),
                             reads=['msq'], writes=[PS(1), 'var'])

                    def s2():
                        P.op('act', lambda e: e.activation(out=var, in_=var, func=AF.Ln, bias=epsg, scale=1.0), reads=['epsv'], writes=['var'])
                        P.op('act', lambda e: e.activation(out=var, in_=var, func=AF.Exp, scale=-0.5), writes=['var'])

                    def mk_sub(a):
                        def f():
                            P.op('dve', lambda e: e.tensor_tensor(out=tt[a], in0=ybf_[:, a, :], in1=mean, op=ALU.subtract),
                                 reads=[(bk_, a), 'mean'], writes=[('tt', a)])
                        return f

                    def mk_mul(a):
                        def f():
                            P.op('dve', lambda e: e.tensor_tensor(out=tt[a], in0=tt[a], in1=var, op=ALU.mult),
                                 reads=['var'], writes=[('tt', a)])
                        return f

                    def mk_out(a):
                        def f():
                            P.op('dve', lambda e: e.scalar_tensor_tensor(
                                out=yo[:, a, :], in0=tt[a], scalar=gnv[:, h * 2 + a:h * 2 + a + 1], in1=sgT[:, a, tsl], op0=ALU.mult, op1=ALU.mult),
                                reads=[('tt', a), 'gnv', ('sgT', a, T)], writes=[(yok, a)])
                        return f

                    def s_dma():
                        dst = yb_d.ap()[h * 256:(h + 1) * 256, tsl].rearrange("(a p) n -> p a n", p=128)
                        P.dma('sp', lambda e: e.dma_start(out=dst, in_=yo), reads=[(yok, 0), (yok, 1)],
                              writes=[('ybuf', h * 2, T), ('ybuf', h * 2 + 1, T)])
                    return [s1, s2, mk_sub(0), mk_mul(0), mk_out(0), mk_sub(1), mk_mul(1), mk_out(1), s_dma]

                pend = []
                stA(0)
                for n in range(16):
                    if n + 1 < 16:
                        stA(n + 1)
                    stB(n)
                    for _ in range(3):
                        if pend:
                            pend.pop(0)()
                    if n % 4 == 3:
                        g1(n // 4)
                        pend += g2_steps(n // 4)
                for st_ in pend:
                    st_()

        def diff_phase(l):
            A.reset()
            lam_init = 0.8 - 0.6 * math.exp(-0.3 * l)
            lp = A.f32(256)
            ltmp = A.f32(64)
            lsc = A.f32(8)
            sgn = A.f32(2)
            QTm = [A.bf(S) for _ in range(2)]
            KT = A.bf(S)
            V = A.bf(16 * 128).rearrange("p (n e) -> p n e", n=16)
            btile = [A.f32(256) for _ in range(2)]
            tmpb = [A.f32(256) for _ in range(2)]
            Pm = [A.bf(512) for _ in range(4)]
            P.op('dve', lambda e: e.memset(QTm[0][64:128, :], 0.0), writes=[('QT', j) for j in range(4)])
            P.op('dve', lambda e: e.memset(QTm[1][0:64, :], 0.0), writes=[('QT', j) for j in range(4)])
            r_ = [[A.f32(512) for _ in range(2)] for _ in range(2)]
            o_ = [[A.f32(512) for _ in range(2)] for _ in range(2)]
            osq_ = [A.bf(512) for _ in range(2)]
            sd_ = [A.f32(512) for _ in range(2)]
            yo_ = [A.bf(512) for _ in range(2)]
            P.dma('sp', lambda e: e.dma_start(out=lp, in_=bcast_row(dlam, l * 256, 256)), writes=['lp'])
            P.dma('sp', lambda e: e.dma_start(out=sgn[:, 0:1], in_=col_vec(subln, l * 128)), writes=['sgn'])
            for i in range(2):
                P.op('dve', lambda e, i=i: e.tensor_tensor(out=ltmp, in0=lp[:, i * 128:i * 128 + 64], in1=lp[:, i * 128 + 64:i * 128 + 128], op=ALU.mult),
                     reads=['lp'], writes=['ltmp'])
                P.op('dve', lambda e, i=i: e.reduce_sum(out=lsc[:, i:i + 1], in_=ltmp, axis=mybir.AxisListType.X), reads=['ltmp'], writes=['lsc'])
                P.op('act', lambda e, i=i: e.activation(out=lsc[:, 2 + i:3 + i], in_=lsc[:, i:i + 1], func=AF.Exp), writes=['lsc'])
            P.op('dve', lambda e: e.tensor_tensor(out=lsc[:, 4:5], in0=lsc[:, 2:3], in1=lsc[:, 3:4], op=ALU.subtract), writes=['lsc'])
            P.op('dve', lambda e: e.tensor_scalar(out=lsc[:, 5:6], in0=lsc[:, 4:5], scalar1=lam_init, scalar2=-1.0, op0=ALU.add, op1=ALU.mult),
                 writes=['lsc'])
            nlam = lsc[:, 5:6]
            P.op('dve', lambda e: e.tensor_scalar(out=sgn[:, 1:2], in0=sgn[:, 0:1], scalar1=1.0 - lam_init, scalar2=None, op0=ALU.mult),
                 reads=[], writes=['sgn'])
            sgv = sgn[:, 1:2]
            wl = w_in.ap()[l]
            for h in diff_heads:
                bt = btile[h % 2]
                btk = ('bt', h % 2)
                P.dma('sp', lambda e, bt=bt, h=h: e.dma_start(out=bt, in_=dbias_d.ap()[h]), writes=[btk])
                cb = cbias[:, h:h + 1]
                wA, kA, oA = wload([wl[:, 3072 + h * 128:3072 + (h + 1) * 128], wl[:, 4096 + h * 128:4096 + (h + 1) * 128],
                                    wl[:, 5120 + h * 128:5120 + (h + 1) * 128]])
                pbanks = (7, 0, 1, 2)
                pc = 0
                for j in range(4):
                    sl = slice(j * 512, (j + 1) * 512)

                    def evq(ps_ap, psk, sl=sl, j=j):
                        P.op('act', lambda e: e.activation(out=QTm[0][0:64, sl], in_=ps_ap[0:64, :], func=AF.Copy, scale=0.125),
                             writes=[psk, ('QT', j)])
                        P.op('dve', lambda e: e.tensor_scalar(out=QTm[1][64:128, sl], in0=ps_ap[64:128, :], scalar1=0.125, scalar2=None, op0=ALU.mult),
                             writes=[psk, ('QT', j)])

                    def evk(ps_ap, psk, sl=sl, j=j):
                        P.op('dve', lambda e: e.tensor_copy(out=KT[:, sl], in_=ps_ap), writes=[psk, ('KT', j)])
                    proj_fm(wA, kA[0], oA[0], 128, j, pbanks[pc % 4], evq)
                    pc += 1
                    proj_fm(wA, kA[1], oA[1], 128, j, pbanks[pc % 4], evk)
                    pc += 1
                for t in range(16):
                    vb = pbanks[pc % 4]
                    pc += 1
                    for c in range(8):
                        P.op('pe', lambda e, c=c, t=t, wA=wA, oA=oA, vb=vb: e.matmul(
                            psum[vb][:, 0:128], lhsT=hT[:, c, t * 128:(t + 1) * 128], rhs=wA[:, c, oA[2]:oA[2] + 128],
                            start=(c == 0), stop=(c == 7)), reads=[kA[2], ('hT', t // 4)], writes=[PS(vb)])
                    if t % 2 == 0:
                        P.op('act', lambda e, t=t, vb=vb: e.activation(out=V[:, t, :], in_=psum[vb][:, 0:128], func=AF.Copy), writes=[PS(vb), ('V', t)])
                    else:
                        P.op('dve', lambda e, t=t, vb=vb: e.tensor_copy(out=V[:, t, :], in_=psum[vb][:, 0:128]), writes=[PS(vb), ('V', t)])
                items = [(Q, j, m) for Q in range(4) for j in range(4 * Q + 4) for m in range(2)]

                def stageA(idx, bt=bt, cb=cb, btk=btk):
                    Q, j, m = items[idx]
                    r = j - 4 * Q
                    q0 = max(r, 0) * 128
                    sbk = idx % 3
                    pm = Pm[idx % 4]
                    pmk = ('Pm', idx % 4)
                    P.op('pe', lambda e: e.matmul(
                        psum[sbk][:, q0:512], lhsT=KT[:, j * 128:(j + 1) * 128], rhs=QTm[m][:, Q * 512 + q0:(Q + 1) * 512],
                        start=True, stop=True), reads=[('KT', j // 4), ('QT', Q)], writes=[PS(sbk)])
                    if r >= -1:
                        if r == -1:
                            s0, s1, b0 = 0, 128, 128
                        else:
                            s0, s1, b0 = q0, min(q0 + 256, 512), 0
                        tb = tmpb[idx % 2]
                        tbk = ('tmpb', idx % 2)
                        w_ = s1 - s0
                        P.op('dve', lambda e: e.tensor_tensor(
                            out=tb[:, 0:w_], in0=psum[sbk][:, s0:s1], in1=bt[:, b0:b0 + w_], op=ALU.add),
                            reads=[btk], writes=[PS(sbk), tbk])
                        P.op('act', lambda e: e.activation(out=pm[:, s0:s1], in_=tb[:, 0:w_], func=AF.Exp),
                             reads=[tbk], writes=[(pmk, 0)])
                        c0 = s1
                    else:
                        c0 = 0
                    if c0 < 512:
                        P.op('act', lambda e: e.activation(out=pm[:, c0:512], in_=psum[sbk][:, c0:512], func=AF.Exp, bias=cb),
                             reads=['cbias'], writes=[PS(sbk), (pmk, 1)])

                def stageB(idx):
                    Q, j, m = items[idx]
                    nj = 4 * Q + 4
                    q0 = max(j - 4 * Q, 0) * 128
                    pm = Pm[idx % 4]
                    pmk = ('Pm', idx % 4)
                    P.op('pe', lambda e: e.matmul(
                        psum[3 + m][:, q0:512], lhsT=V[:, j, :], rhs=pm[:, q0:512], start=(j == 0), stop=(j == nj - 1)),
                        reads=[('V', j), (pmk, 0), (pmk, 1)], writes=[PS(3 + m)])
                    P.op('pe', lambda e: e.matmul(
                        psum[5 + m][:, q0:512], lhsT=ones, rhs=pm[:, q0:512], start=(j == 0), stop=(j == nj - 1)),
                        reads=['cst', (pmk, 0), (pmk, 1)], writes=[PS(5 + m)])

                def fin_steps(Q, h=h):
                    rr = r_[Q % 2]
                    oo = o_[Q % 2]
                    osq = osq_[Q % 2]
                    sd = sd_[Q % 2]
                    qk = Q % 2
                    yo = yo_[Q % 2]
                    yok = ('yo', Q % 2)
                    r0 = 1024 + h * 128
                    for m in range(2):
                        P.op('dve', lambda e, m=m: e.tensor_copy(out=rr[m], in_=psum[5 + m][:, :]), writes=[PS(5 + m), ('r', qk, m)])
                        P.op('dve', lambda e, m=m: e.tensor_copy(out=oo[m], in_=psum[3 + m][:, :]), writes=[PS(3 + m), ('o', qk, m)])
                    steps = []
                    for m in range(2):
                        steps.append(lambda m=m: P.op('act', lambda e: e.activation(out=rr[m], in_=rr[m], func=AF.Ln), writes=[('r', qk, m)]))
                        steps.append(lambda m=m: P.op('act', lambda e: e.activation(out=rr[m], in_=rr[m], func=AF.Exp, scale=-1.0), writes=[('r', qk, m)]))
                        steps.append(lambda m=m: P.op('dve', lambda e: e.tensor_tensor(out=oo[m], in0=oo[m], in1=rr[m], op=ALU.mult),
                                                      reads=[('r', qk, m)], writes=[('o', qk, m)]))

                    def s7():
                        P.op('dve', lambda e: e.scalar_tensor_tensor(out=oo[0], in0=oo[1], scalar=nlam, in1=oo[0], op0=ALU.mult, op1=ALU.add),
                             reads=[('o', qk, 1), 'lsc'], writes=[('o', qk, 0)])
                        P.op('act', lambda e: e.activation(out=osq, in_=oo[0], func=AF.Square), reads=[('o', qk, 0)], writes=[('osq', qk)])

                    def s8():
                        P.op('pe', lambda e: e.matmul(psum[7][:, :], lhsT=ones, rhs=osq, start=True, stop=True), reads=[('osq', qk), 'cst'], writes=[PS(7)])
                        P.op('act', lambda e: e.activation(out=sd, in_=psum[7][:, :], func=AF.Ln, bias=epsd, scale=1.0 / 128), reads=['epsv'],
                             writes=[PS(7), ('sd', qk)])

                    def s9():
                        P.op('act', lambda e: e.activation(out=sd, in_=sd, func=AF.Exp, scale=-0.5), writes=[('sd', qk)])
                        P.op('dve', lambda e: e.scalar_tensor_tensor(out=yo, in0=oo[0], scalar=sgv, in1=sd, op0=ALU.mult, op1=ALU.mult),
                             reads=[('o', qk, 0), ('sd', qk), 'sgn'], writes=[yok])
                        P.dma('sp', lambda e: e.dma_start(out=yb_d.ap()[r0:r0 + 128, Q * 512:(Q + 1) * 512], in_=yo),
                              reads=[yok], writes=[('ybuf', r0 // 128, Q)])
                    steps += [s7, s8, s9]
                    return steps

                LOOK = 2
                n_it = len(items)
                pending = []
                for i in range(min(LOOK, n_it)):
                    stageA(i)
                for i in range(n_it):
                    if i + LOOK < n_it:
                        stageA(i + LOOK)
                    stageB(i)
                    Q, j, m = items[i]
                    if pending:
                        pending.pop(0)()
                    if j == 4 * Q + 3 and m == 1:
                        for st_ in pending:
                            st_()
                        pending = fin_steps(Q)
                for st_ in pending:
                    st_()

        def dil_phase(l):
            A.reset()
            QT = [A.bf(S) for _ in range(2)]
            KT = [A.bf(S) for _ in range(2)]
            Vs = [A.bf(16 * 128).rearrange("p (n e) -> p n e", n=16) for _ in range(2)]
            btile = [A.f32(256) for _ in range(2)]
            tmpb = [A.f32(256) for _ in range(2)]
            Pm = [A.bf(256) for _ in range(4)]
            acc = A.f32(2 * S).rearrange("p (a n) -> p a n", a=2)
            yo = A.bf(S)
            wl = w_in.ap()[l]
            gi = 0
            for hs in dil_slots:
                for g, (win, dil) in enumerate(DIL):
                    if g not in DILG:
                        continue
                    nb = S // dil // 128
                    qt = QT[gi % 2]
                    kt = KT[gi % 2]
                    vs = Vs[gi % 2]
                    bt = btile[gi % 2]
                    gk = gi % 2
                    gi += 1
                    P.dma('sp', lambda e, bt=bt, g=g, hs=hs: e.dma_start(out=bt, in_=lbias_d.ap()[g * 4 + hs]), writes=[('bt', gk)])
                    base = 6144 + g * 1536 + hs * 128
                    wA, kA, oA = wload([wl[:, base:base + 128], wl[:, base + 512:base + 640], wl[:, base + 1024:base + 1152]])
                    for j in range(4):
                        sl = slice(j * 512, (j + 1) * 512)

                        def evq(ps_ap, psk, sl=sl, j=j, qt=qt):
                            P.op('act', lambda e: e.activation(out=qt[:, sl], in_=ps_ap, func=AF.Copy), writes=[psk, ('QT', gk, j)])

                        def evk(ps_ap, psk, sl=sl, j=j, kt=kt):
                            P.op('dve', lambda e: e.tensor_copy(out=kt[:, sl], in_=ps_ap), writes=[psk, ('KT', gk, j)])
                        proj_fm(wA, kA[0], oA[0], 128, j, 0, evq)
                        proj_fm(wA, kA[1], oA[1], 128, j, 1, evk)
                    HTall = [('hT', j) for j in range(4)]
                    for r in range(dil):
                        for jb in range(nb):
                            ti = r * nb + jb
                            t0 = r + dil * jb * 128
                            tsl = slice(t0, t0 + dil * 127 + 1, dil)
                            bank = ti % 2
                            for c in range(8):
                                P.op('pe', lambda e, c=c, tsl=tsl, bank=bank, wA=wA, oA=oA: e.matmul(
                                    psum[bank][:, 0:128], lhsT=hT[:, c, tsl], rhs=wA[:, c, oA[2]:oA[2] + 128],
                                    start=(c == 0), stop=(c == 7)), reads=[kA[2]] + HTall, writes=[PS(bank)])
                            P.op('act', lambda e, ti=ti, bank=bank, vs=vs: e.activation(out=vs[:, ti, :], in_=psum[bank][:, 0:128], func=AF.Copy),
                                 writes=[PS(bank), ('V', gk, ti)])
                    scale = 128.0 ** -0.5
                    QTall = [('QT', gk, j) for j in range(4)]
                    KTall = [('KT', gk, j) for j in range(4)]
                    items = [(r, jb) for r in range(dil) for jb in range(nb)]
                    first_g = (g == DILG[0])

                    def stageA(idx, kt=kt, qt=qt, bt=bt, gk=gk, dil=dil, nb=nb, items=items, QTall=QTall, KTall=KTall):
                        r, jb = items[idx]
                        nq = 256 if jb < nb - 1 else 128
                        t0 = r + dil * jb * 128
                        ksl = slice(t0, t0 + dil * 127 + 1, dil)
                        qsl = slice(t0, t0 + dil * (nq - 1) + 1, dil)
                        sbk = 2 + idx % 3
                        pm = Pm[idx % 4]
                        pmk = ('Pm', idx % 4)
                        tb = tmpb[idx % 2]
                        tbk = ('tmpb', idx % 2)
                        P.op('pe', lambda e: e.matmul(psum[sbk][:, 0:nq], lhsT=kt[:, ksl], rhs=qt[:, qsl], start=True, stop=True),
                             reads=QTall + KTall, writes=[PS(sbk)])
                        P.op('dve', lambda e: e.scalar_tensor_tensor(
                            out=tb[:, 0:nq], in0=psum[sbk][:, 0:nq], scalar=scale, in1=bt[:, 0:nq], op0=ALU.mult, op1=ALU.add),
                            reads=[('bt', gk)], writes=[PS(sbk), tbk])
                        P.op('act', lambda e: e.activation(out=pm[:, 0:nq], in_=tb[:, 0:nq], func=AF.Exp), reads=[tbk], writes=[pmk])

                    def stageB(idx, vs=vs, gk=gk, dil=dil, nb=nb, items=items, first_g=first_g):
                        r, jb = items[idx]
                        ti = r * nb + jb
                        t0 = r + dil * jb * 128
                        pm = Pm[idx % 4]
                        pmk = ('Pm', idx % 4)
                        nbk = 5 + idx % 3
                        rd = [('V', gk, ti), pmk, 'cst']
                        has_prev = jb > 0
                        if has_prev:
                            ppm = Pm[(idx - 1) % 4]
                            ppmk = ('Pm', (idx - 1) % 4)
                            pti = ti - 1
                            rd += [ppmk, ('V', gk, pti)]
                            P.op('pe', lambda e: e.matmul(psum[nbk][:, 0:128], lhsT=vs[:, pti, :], rhs=ppm[:, 128:256], start=True, stop=False),
                                 reads=rd, writes=[PS(nbk)])
                            P.op('pe', lambda e: e.matmul(psum[nbk][:, 128:256], lhsT=ones, rhs=ppm[:, 128:256], start=False, stop=False),
                                 reads=rd, writes=[PS(nbk)])
                        P.op('pe', lambda e: e.matmul(psum[nbk][:, 0:128], lhsT=vs[:, ti, :], rhs=pm[:, 0:128], start=(not has_prev), stop=False),
                             reads=rd, writes=[PS(nbk)])
                        P.op('pe', lambda e: e.matmul(psum[nbk][:, 128:256], lhsT=ones, rhs=pm[:, 0:128], start=False, stop=True),
                             reads=rd, writes=[PS(nbk)])
                        asl = slice(t0, t0 + dil * 127 + 1, dil)
                        src = psum[nbk][:, 0:256].rearrange("p (a n) -> p a n", a=2)
                        if first_g:
                            P.op('dve', lambda e: e.tensor_copy(out=acc[:, :, asl], in_=src), writes=[PS(nbk), 'acc'])
                        else:
                            P.op('dve', lambda e: e.tensor_tensor(out=acc[:, :, asl], in0=src, in1=acc[:, :, asl], op=ALU.add),
                                 writes=[PS(nbk), 'acc'])

                    LOOK = 2
                    n_it = len(items)
                    for i in range(min(LOOK, n_it)):
                        stageA(i)
                    for i in range(n_it):
                        if i + LOOK < n_it:
                            stageA(i + LOOK)
                        stageB(i)
                P.op('act', lambda e: e.activation(out=acc[:, 1, :], in_=acc[:, 1, :], func=AF.Ln), writes=['acc'])
                P.op('act', lambda e: e.activation(out=acc[:, 1, :], in_=acc[:, 1, :], func=AF.Exp, scale=-1.0), writes=['acc'])
                P.op('dve', lambda e: e.tensor_tensor(out=yo, in0=acc[:, 0, :], in1=acc[:, 1, :], op=ALU.mult), reads=['acc'], writes=['yo'])
                r0 = 2048 + hs * 128
                P.dma('sp', lambda e, r0=r0: e.dma_start(out=yb_d.ap()[r0:r0 + 128, :], in_=yo), reads=['yo'],
                      writes=[('ybuf', r0 // 128, Q) for Q in range(4)])

        def merge_phase(l, xsrc, xdst):
            A.reset()
            yts = [A.bf(20 * 512).rearrange("p (c n) -> p c n", c=20) for _ in range(2)]
            sg = [A.f32(512) for _ in range(2)]
            tmp = [A.f32(512) for _ in range(2)]
            macc = A.f32(8 * 512).rearrange("p (c n) -> p c n", c=8)
            mbf = A.bf(8 * 512).rearrange("p (c n) -> p c n", c=8)
            xt = [A.f32(D) for _ in range(4)]
            wl = w_in.ap()[l]
            branches = [(w_bret.ap()[l], 0, 8), (w_bdiff.ap()[l], 8, 8), (w_bdil.ap()[l], 16, 4)]

            def load_y(T):
                yt = yts[T % 2]
                tsl = slice(T * 512, (T + 1) * 512)
                src = yb_d.ap()[:, tsl].rearrange("(c p) n -> p c n", p=128)
                P.dma('sp', lambda e: e.dma_start(out=yt[:, 0:10, :], in_=src[:, 0:10, :]),
                      reads=[('ybuf', c, T) for c in range(10)], writes=[('yt', T % 2, 0)])
                P.dma('sp', lambda e: e.dma_start(out=yt[:, 10:20, :], in_=src[:, 10:20, :]),
                      reads=[('ybuf', c, T) for c in range(10, 20)], writes=[('yt', T % 2, 1)])

            load_y(0)
            for T in range(4):
                tsl = slice(T * 512, (T + 1) * 512)
                yt = yts[T % 2]
                ytk = [('yt', T % 2, 0), ('yt', T % 2, 1)]
                if T + 1 < 4:
                    load_y(T + 1)
                for ts in range(4):
                    t = T * 4 + ts
                    P.dma('sp', lambda e, ts=ts, t=t: e.dma_start(out=xt[ts], in_=xsrc[t * 128:(t + 1) * 128, :]), writes=[('xt', ts)])
                cnt = 0
                for bi, (wb, c0, nch) in enumerate(branches):
                    for hf in range(2):
                        wB, kB, oB = wload([wb[:, hf * 512:(hf + 1) * 512]])
                        gcol = 10752 + bi * 1024 + hf * 512
                        wG, kG, oG = wload([wl[:, gcol:gcol + 512]])
                        for d4 in range(4):
                            d = hf * 4 + d4
                            zb = cnt % 2
                            gb = 2 + cnt % 2
                            s_ = sg[cnt % 2]
                            sk = ('sg', cnt % 2)
                            tm = tmp[cnt % 2]
                            tmk = ('tmp', cnt % 2)
                            cnt += 1
                            for c in range(nch):
                                P.op('pe', lambda e, c=c, zb=zb, wB=wB, d4=d4, c0=c0, nch=nch, yt=yt: e.matmul(
                                    psum[zb][:, :], lhsT=wB[:, c, d4 * 128:(d4 + 1) * 128], rhs=yt[:, c0 + c, :],
                                    start=(c == 0), stop=(c == nch - 1)), reads=[kB[0]] + ytk, writes=[PS(zb)])
                            for c in range(8):
                                P.op('pe', lambda e, c=c, gb=gb, wG=wG, d4=d4, tsl=tsl: e.matmul(
                                    psum[gb][:, :], lhsT=wG[:, c, d4 * 128:(d4 + 1) * 128], rhs=hT[:, c, tsl],
                                    start=(c == 0), stop=(c == 7)), reads=[kG[0], ('hT', T)], writes=[PS(gb)])
                            P.op('act', lambda e, gb=gb, s_=s_: e.activation(out=s_, in_=psum[gb][:, :], func=AF.Sigmoid), writes=[PS(gb), sk])
                            if bi == 0:
                                P.op('dve', lambda e, zb=zb, s_=s_, d=d: e.tensor_tensor(out=macc[:, d, :], in0=psum[zb][:, :], in1=s_, op=ALU.mult),
                                     reads=[sk], writes=[PS(zb), ('macc', d)])
                            else:
                                P.op('dve', lambda e, zb=zb, s_=s_, tm=tm: e.tensor_tensor(out=tm, in0=psum[zb][:, :], in1=s_, op=ALU.mult),
                                     reads=[sk], writes=[PS(zb), tmk])
                                if bi == 1:
                                    P.op('dve', lambda e, tm=tm, d=d: e.tensor_tensor(out=macc[:, d, :], in0=macc[:, d, :], in1=tm, op=ALU.add),
                                         reads=[tmk], writes=[('macc', d)])
                                else:
                                    P.op('dve', lambda e, tm=tm, d=d: e.tensor_tensor(out=mbf[:, d, :], in0=macc[:, d, :], in1=tm, op=ALU.add),
                                         reads=[tmk, ('macc', d)], writes=[('mbf', d)])
                for ch in range(2):
                    wO, kO, oO = wload([w_out.ap()[l][:, ch * 512:(ch + 1) * 512]])
                    for ts in range(4):
                        bank = 4 + ts
                        for d in range(8):
                            P.op('pe', lambda e, d=d, ts=ts, bank=bank, wO=wO: e.matmul(
                                psum[bank][:, :], lhsT=mbf[:, d, ts * 128:(ts + 1) * 128], rhs=wO[:, d, 0:512],
                                start=(d == 0), stop=(d == 7)), reads=[kO[0], ('mbf', d)], writes=[PS(bank)])
                        P.op('dve', lambda e, ts=ts, ch=ch, bank=bank: e.tensor_tensor(
                            out=xt[ts][:, ch * 512:(ch + 1) * 512], in0=psum[bank][:, :], in1=xt[ts][:, ch * 512:(ch + 1) * 512], op=ALU.add),
                            writes=[PS(bank), ('xt', ts)])
                for ts in range(4):
                    t = T * 4 + ts
                    P.dma('sp', lambda e, ts=ts, t=t: e.dma_start(out=xdst[t * 128:(t + 1) * 128, :], in_=xt[ts]), reads=[('xt', ts)],
                          writes=[('xs', t)])

        def ffn_phase(l, xsd, nxt, odst):
            A.reset()
            xts = [[A.f32(D) for _ in range(4)] for _ in range(2)]
            sq = A.f32(D)
            hn = [A.bf(D) for _ in range(2)]
            gbc = A.f32(D)
            h2Ts = [A.bf(8 * 512).rearrange("p (c n) -> p c n", c=8) for _ in range(2)]
            aT = A.bf(22 * 512).rearrange("p (c n) -> p c n", c=22)
            sg = [A.f32(512) for _ in range(2)]
            P.dma('sp', lambda e: e.dma_start(out=gbc, in_=bcast_row(norm_ffn, l * D, D)), writes=['gbc'])
            gbc2 = A.f32(D)
            if nxt[0] == 'hT':
                hne = [A.bf(D) for _ in range(4)]
                P.dma('sp', lambda e: e.dma_start(out=gbc2, in_=bcast_row(norm_mix, nxt[1] * D, D)), writes=['gbc2'])
            else:
                ote = [A.f32(D) for _ in range(2)]
                P.dma('sp', lambda e: e.dma_start(out=gbc2, in_=bcast_row(norm_final, 0, D)), writes=['gbc2'])

            def epi_elem(T, xt):
                for ts in range(4):
                    t = T * 4 + ts
                    xk = ('xt', T % 2, ts)
                    rs, k = rms_stats(xt[ts], xk, sq, 16 + ts, RMS_EPS)
                    if nxt[0] == 'hT':
                        P.op('dve', lambda e, ts=ts, rs=rs: e.scalar_tensor_tensor(out=hne[ts], in0=xt[ts], scalar=rs, in1=gbc2,
                                                                                     op0=ALU.mult, op1=ALU.mult),
                             reads=[xk, k, 'gbc2'], writes=[('hne', ts)])
                    else:
                        ob = ote[t % 2]
                        P.op('dve', lambda e, ts=ts, rs=rs, ob=ob: e.scalar_tensor_tensor(out=ob, in0=xt[ts], scalar=rs, in1=gbc2,
                                                                                        op0=ALU.mult, op1=ALU.mult),
                             reads=[xk, k, 'gbc2'], writes=[('ote', t % 2)])
                        P.dma('sp', lambda e, ob=ob, t=t: e.dma_start(out=odst[t * 128:(t + 1) * 128, :], in_=ob), reads=[('ote', t % 2)],
                              is_output=True)

            def epi_tr(T):
                if nxt[0] != 'hT':
                    return
                pv = psum_bf[7][:, 0:1024].rearrange("p (c n) -> p c n", c=8)
                for ts in range(4):
                    t = T * 4 + ts
                    for c in range(8):
                        P.op('pe', lambda e, c=c, ts=ts: e.transpose(out=pv[:, c, :], in_=hne[ts][:, c * 128:(c + 1) * 128], identity=ident),
                             reads=[('hne', ts), 'cst'], writes=[PS(7)])
                    P.op('act', lambda e, t=t: e.activation(out=hT[:, :, t * 128:(t + 1) * 128], in_=pv, func=AF.Copy),
                         writes=[PS(7), ('hT', t // 4)])
            wg = w_g.ap()[l]
            wu = w_u.ap()[l]
            wd = w_d.ap()[l]

            def load_x(T):
                for ts in range(4):
                    t = T * 4 + ts
                    P.dma('sp', lambda e, ts=ts, t=t: e.dma_start(out=xts[T % 2][ts], in_=xsd[t * 128:(t + 1) * 128, :]),
                          reads=[('xs', t)], writes=[('xt', T % 2, ts)])

            def norm_elem(T, ts):
                t = T * 4 + ts
                xk = ('xt', T % 2, ts)
                xb = xts[T % 2][ts]
                rs, k = rms_stats(xb, xk, sq, 8 + (t % 8), RMS_EPS)
                hb = hn[t % 2]
                hk = ('hn', t % 2)
                P.op('dve', lambda e: e.scalar_tensor_tensor(out=hb, in0=xb, scalar=rs, in1=gbc, op0=ALU.mult, op1=ALU.mult),
                     reads=[xk, k, 'gbc'], writes=[hk])

            def norm_tr(T, ts):
                t = T * 4 + ts
                hb = hn[t % 2]
                hk = ('hn', t % 2)
                h2T = h2Ts[T % 2]
                pv = psum_bf[7][:, 0:1024].rearrange("p (c n) -> p c n", c=8)
                for c in range(8):
                    P.op('pe', lambda e, c=c: e.transpose(out=pv[:, c, :], in_=hb[:, c * 128:(c + 1) * 128], identity=ident),
                         reads=[hk, 'cst'], writes=[PS(7)])
                P.op('act', lambda e: e.activation(out=h2T[:, :, ts * 128:(ts + 1) * 128], in_=pv, func=AF.Copy),
                     writes=[PS(7), ('h2T', T % 2)])

            load_x(0)
            for ts in range(4):
                norm_elem(0, ts)
                norm_tr(0, ts)
            for T in range(4):
                xt = xts[T % 2]
                h2T = h2Ts[T % 2]
                h2k = ('h2T', T % 2)
                if T + 1 < 4:
                    load_x(T + 1)
                cnt = 0
                for gi_, f0 in enumerate(range(0, FFN, 512)):
                    nf = min(512, FFN - f0)
                    wG, kG, oG = wload([wg[:, f0:f0 + nf]])
                    wU, kU, oU = wload([wu[:, f0:f0 + nf]])
                    for f4 in range(nf // 128):
                        fc = f0 // 128 + f4
                        gb = cnt % 2
                        ub = 2 + cnt % 2
                        s_ = sg[cnt % 2]
                        sk = ('sg', cnt % 2)
                        cnt += 1
                        for c in range(8):
                            P.op('pe', lambda e, c=c, gb=gb, wG=wG, f4=f4, h2T=h2T: e.matmul(
                                psum[gb][:, :], lhsT=wG[:, c, f4 * 128:(f4 + 1) * 128], rhs=h2T[:, c, :], start=(c == 0), stop=(c == 7)),
                                reads=[kG[0], h2k], writes=[PS(gb)])
                        for c in range(8):
                            P.op('pe', lambda e, c=c, ub=ub, wU=wU, f4=f4, h2T=h2T: e.matmul(
                                psum[ub][:, :], lhsT=wU[:, c, f4 * 128:(f4 + 1) * 128], rhs=h2T[:, c, :], start=(c == 0), stop=(c == 7)),
                                reads=[kU[0], h2k], writes=[PS(ub)])
                        P.op('act', lambda e, gb=gb, s_=s_: e.activation(out=s_, in_=psum[gb][:, :], func=AF.Silu), writes=[PS(gb), sk])
                        P.op('dve', lambda e, ub=ub, s_=s_, fc=fc: e.tensor_tensor(out=aT[:, fc, :], in0=psum[ub][:, :], in1=s_, op=ALU.mult),
                             reads=[sk], writes=[PS(ub), ('aT', fc)])
                    if gi_ == 0 and T > 0:
                        epi_tr(T - 1)
                    if T + 1 < 4:
                        if gi_ < 4:
                            norm_elem(T + 1, gi_)
                        if 1 <= gi_ <= 4:
                            norm_tr(T + 1, gi_ - 1)
                for ch in range(2):
                    for g3, (fa, fb) in enumerate(((0, 8), (8, 16), (16, 22))):
                        nch = fb - fa
                        wD, kD, oD = wload([wd[fa * 128:fb * 128, ch * 512:(ch + 1) * 512]])
                        for ts in range(4):
                            bank = 4 + ts
                            for c in range(nch):
                                fc = fa + c
                                P.op('pe', lambda e, c=c, fc=fc, ts=ts, bank=bank, wD=wD: e.matmul(
                                    psum[bank][:, :], lhsT=aT[:, fc, ts * 128:(ts + 1) * 128], rhs=wD[:, c, 0:512],
                                    start=(fc == 0), stop=(fc == 21)), reads=[kD[0], ('aT', fc)], writes=[PS(bank)])
                    for ts in range(4):
                        bank = 4 + ts
                        P.op('dve', lambda e, ts=ts, ch=ch, bank=bank, xt=xt: e.tensor_tensor(
                            out=xt[ts][:, ch * 512:(ch + 1) * 512], in0=psum[bank][:, :], in1=xt[ts][:, ch * 512:(ch + 1) * 512], op=ALU.add),
                            writes=[PS(bank), ('xt', T % 2, ts)])
                if nxt[0] == 'hT':
                    for ts in range(4):
                        t = T * 4 + ts
                        P.dma('sp', lambda e, ts=ts, t=t, xt=xt: e.dma_start(out=xsd[t * 128:(t + 1) * 128, :], in_=xt[ts]),
                              reads=[('xt', T % 2, ts)], writes=[('xs', t)])
                epi_elem(T, xt)
            epi_tr(3)

        def final_phase(xsd, odst):
            A.reset()
            xt = [A.f32(D) for _ in range(3)]
            sq = A.f32(D)
            ot = [A.f32(D) for _ in range(2)]
            gbc = A.f32(D)
            P.dma('sp', lambda e: e.dma_start(out=gbc, in_=bcast_row(norm_final, 0, D)), writes=['gbc'])
            for t in range(16):
                xb = xt[t % 3]
                xk = ('xt', t % 3)
                P.dma('sp', lambda e, xb=xb, t=t: e.dma_start(out=xb, in_=xsd[t * 128:(t + 1) * 128, :]), reads=[('xs', t)], writes=[xk])
                rs, k = rms_stats(xb, xk, sq, t % 8, RMS_EPS)
                ob = ot[t % 2]
                ok_ = ('ot', t % 2)
                P.op('dve', lambda e, ob=ob, xb=xb, rs=rs: e.scalar_tensor_tensor(
                    out=ob, in0=xb, scalar=rs, in1=gbc, op0=ALU.mult, op1=ALU.mult), reads=[xk, k, 'gbc'], writes=[ok_])
                P.dma('sp', lambda e, ob=ob, t=t: e.dma_start(out=odst[t * 128:(t + 1) * 128, :], in_=ob), reads=[ok_], is_output=True)

        for s in range(n_seq):
            for li, l in enumerate(layers):
                xin = x_d.ap()[s] if li == 0 else xs_d.ap()[s]
                xsd = xs_d.ap()[s]
                if "n1" in phases and (li == 0 or "ffn" not in phases):
                    norm_phase_hT(xin, norm_mix, l * D)
                    P.fence()
                if "ret" in phases:
                    retention_phase(l)
                    P.fence()
                if "diff" in phases:
                    diff_phase(l)
                    P.fence()
                if "dil" in phases:
                    dil_phase(l)
                    P.fence()
                if "merge" in phases:
                    merge_phase(l, xin, xsd)
                    P.fence()
                if "ffn" in phases:
                    last = (li == len(layers) - 1)
                    ffn_phase(l, xsd, ('final',) if last else ('hT', layers[li + 1]), out_d.ap()[s])
                    P.fence()
        P.finish()
        P.emit()
    return nc, P


WNAMES = ["w_in", "w_branch_ret", "w_branch_diff", "w_branch_dil", "w_out", "norm_mix", "norm_ffn", "ret_gn_gain",
          "diff_lambda", "diff_subln_gain", "w_ffn_gate", "w_ffn_up", "w_ffn_down", "norm_final"]


def make_in_maps(inputs, n_cores=NCORES, n_seq=SEQ_PER_CORE):
    consts = _host_consts(inputs["rel_bias"])
    shared = {k: np.ascontiguousarray(np.asarray(inputs[k], dtype=np.float32)) for k in WNAMES}
    shared.update(consts)
    x = np.asarray(inputs["x"], dtype=np.float32)
    maps = []
    for i in range(n_cores):
        m = dict(shared)
        m["x"] = np.ascontiguousarray(x[i * n_seq:(i + 1) * n_seq])
        maps.append(m)
    return maps


def kernel(**inputs):
    nc, P = build_program()
    maps = make_in_maps(inputs)
    res = run_bass_kernel_spmd(nc, maps, core_ids=list(range(NCORES)))
    out = np.concatenate([np.asarray(r["out"]) for r in res.results], axis=0)
    return out.astype(np.float32)
```
